# Optimizing a Trainium2 kernel written in Bass

```python
import jax, jax.numpy as jnp
from jax import lax
import numpy as np

D_MODEL = 1024
BATCH = 2
SEQ = 8192
DEPTH = 1

GRID_W = 64
CTX_LEN = 256
D_MIX = D_MODEL
RET_HEADS = 4
RET_WIDTH = D_MIX // 2
RET_DK = RET_WIDTH // RET_HEADS
RET_DV = RET_WIDTH // RET_HEADS
GLA_HEADS = 4
GLA_WIDTH = D_MIX - RET_WIDTH
GLA_DV = GLA_WIDTH // GLA_HEADS
GLA_DK = GLA_DV // 2
GLA_RANK = 16
GLA_GATE_NORM = 16.0
CHUNK = 64
ROPE_BASE = 10000.0
N_KEYS = 128
N_EXPERTS = N_KEYS * N_KEYS
PEER_HEADS = 8
PEER_TOPK = 16
PEER_DQ = 256
PEER_BLOCK = 128
N_MOD = 6
EPS = 1e-6
IN_SIZES = (RET_WIDTH, RET_WIDTH, RET_WIDTH, RET_WIDTH,
            GLA_HEADS * GLA_DK, GLA_HEADS * GLA_DK, GLA_WIDTH, GLA_WIDTH,
            GLA_RANK, GLA_RANK)
IN_COLS = sum(IN_SIZES)

kernel_name = "hymba_retention_gla_peer_dit"

F32 = jnp.float32


def rmsnorm(x, w):
    xf = x.astype(F32)
    y = xf * lax.rsqrt(jnp.mean(xf * xf, axis=-1, keepdims=True) + EPS)
    return (y * w.astype(F32)).astype(x.dtype)


def head_rmsnorm(o):
    o = o * lax.rsqrt(jnp.mean(o * o, axis=-1, keepdims=True) + EPS)
    B, H, L, d = o.shape
    return o.transpose(0, 2, 1, 3).reshape(B, L, H * d)


def modulate(h, shift, scale):
    return h * (1 + scale) + shift


def to_heads(t, n_heads):
    B, L, W = t.shape
    return t.reshape(B, L, n_heads, W // n_heads).transpose(0, 2, 1, 3)


def flip_seq(t):
    return jnp.flip(t, axis=2)


def axial_rope(rows, cols):
    r, c = jnp.meshgrid(jnp.arange(rows, dtype=F32), jnp.arange(cols, dtype=F32), indexing="ij")
    n_freq = RET_DK // 4
    inv = ROPE_BASE ** (-jnp.arange(n_freq, dtype=F32) / n_freq)
    ang = jnp.concatenate([r.reshape(-1, 1) * inv, c.reshape(-1, 1) * inv], axis=-1)
    return jnp.cos(ang), jnp.sin(ang)


def apply_rope(t, cos, sin):
    t1, t2 = jnp.split(t, 2, axis=-1)
    return jnp.concatenate([t1 * cos - t2 * sin, t2 * cos + t1 * sin], axis=-1)


def to_chunks(t):
    B, H, L, d = t.shape
    return t.reshape(B, H, L // CHUNK, CHUNK, d).transpose(2, 0, 1, 3, 4)


def from_chunks(t):
    n, B, H, C, d = t.shape
    return t.transpose(1, 2, 0, 3, 4).reshape(B, H, n * C, d)


def retention_scan(q, k, v, log_gamma, s0):
    idx = jnp.arange(CHUNK, dtype=F32)
    rel = idx[:, None] - idx[None, :]
    lg = log_gamma[:, None, None]
    decay_mask = jnp.where(rel >= 0, jnp.exp(lg * jnp.maximum(rel, 0.0)), 0.0)
    q_dec = jnp.exp(log_gamma[:, None] * (idx + 1.0))[None, :, :, None]
    k_dec = jnp.exp(log_gamma[:, None] * (CHUNK - 1.0 - idx))[None, :, :, None]
    chunk_dec = jnp.exp(log_gamma * CHUNK)[None, :, None, None]

    def step(s, inp):
        qc, kc, vc = inp
        scores = jnp.einsum("bhid,bhjd->bhij", qc, kc) * decay_mask
        out = (jnp.einsum("bhij,bhjv->bhiv", scores, vc)
               + jnp.einsum("bhid,bhdv->bhiv", qc * q_dec, s))
        s = chunk_dec * s + jnp.einsum("bhjd,bhjv->bhdv", kc * k_dec, vc)
        return s, out

    s, out = lax.scan(step, s0, (to_chunks(q), to_chunks(k), to_chunks(v)))
    return from_chunks(out), s


def gla_scan(q, k, v, log_a, s0):
    causal = (jnp.arange(CHUNK)[:, None] >= jnp.arange(CHUNK)[None, :])

    def step(s, inp):
        qc, kc, vc, ac = inp
        G = jnp.cumsum(ac, axis=2)
        G_last = G[:, :, -1:, :]
        q_t = qc * jnp.exp(G)
        k_t = kc * jnp.exp(-G)
        scores = jnp.where(causal, jnp.einsum("bhid,bhjd->bhij", q_t, k_t), 0.0)
        out = (jnp.einsum("bhij,bhjv->bhiv", scores, vc)
               + jnp.einsum("bhid,bhdv->bhiv", q_t, s))
        s = (jnp.exp(G_last[:, :, 0, :])[..., None] * s
             + jnp.einsum("bhjd,bhjv->bhdv", kc * jnp.exp(G_last - G), vc))
        return s, out

    s, out = lax.scan(step, s0, (to_chunks(q), to_chunks(k), to_chunks(v), to_chunks(log_a)))
    return from_chunks(out), s


def retention_final_state(k, v, log_gamma):
    L = k.shape[2]
    w = jnp.exp(log_gamma[:, None] * (L - 1.0 - jnp.arange(L, dtype=F32)))
    return jnp.einsum("bhld,bhle->bhde", k * w[None, :, :, None], v)


def gla_final_state(k, v, log_a):
    G = jnp.cumsum(log_a, axis=2)
    return jnp.einsum("bhld,bhle->bhde", k * jnp.exp(G[:, :, -1:, :] - G), v)


def project(h, w_in, gk_up_f, gk_bias_f, gk_up_b, gk_bias_b, rope):
    p = (h @ w_in).astype(F32)
    splits = [int(s) for s in np.cumsum(IN_SIZES)[:-1]]
    rq, rk, rv, rg, gq, gk, gv, gg, lr_f, lr_b = jnp.split(p, splits, axis=-1)
    rq = to_heads(rq, RET_HEADS)
    rk = to_heads(rk, RET_HEADS) * (RET_DK ** -0.5)
    rv = to_heads(rv, RET_HEADS)
    if rope is not None:
        rq = apply_rope(rq, *rope)
        rk = apply_rope(rk, *rope)
    gq = to_heads(gq, GLA_HEADS) * (GLA_DK ** -0.5)
    gk = to_heads(gk, GLA_HEADS)
    gv = to_heads(gv, GLA_HEADS)
    la_f = to_heads(jax.nn.log_sigmoid(lr_f @ gk_up_f.astype(F32) + gk_bias_f.astype(F32)) / GLA_GATE_NORM, GLA_HEADS)
    la_b = to_heads(jax.nn.log_sigmoid(lr_b @ gk_up_b.astype(F32) + gk_bias_b.astype(F32)) / GLA_GATE_NORM, GLA_HEADS)
    return (rq, rk, rv, rg, gq, gk, gv, gg, la_f, la_b)


def bidir_mix(feats, lg_f, lg_b, states):
    rq, rk, rv, rg, gq, gk, gv, gg, la_f, la_b = feats
    s_rf, s_rb, s_gf, s_gb = states
    o_rf, s_rf = retention_scan(rq, rk, rv, lg_f, s_rf)
    o_rb, s_rb = retention_scan(flip_seq(rq), flip_seq(rk), flip_seq(rv), lg_b, s_rb)
    o_gf, s_gf = gla_scan(gq, gk, gv, la_f, s_gf)
    o_gb, s_gb = gla_scan(flip_seq(gq), flip_seq(gk), flip_seq(gv), flip_seq(la_b), s_gb)
    ret = head_rmsnorm(o_rf + flip_seq(o_rb)) * jax.nn.silu(rg)
    gla = head_rmsnorm(o_gf + flip_seq(o_gb)) * jax.nn.silu(gg)
    return jnp.concatenate([ret, gla], axis=-1), (s_rf, s_rb, s_gf, s_gb)


def context_final_states(feats, lg_f, lg_b):
    _, rk, rv, _, _, gk, gv, _, la_f, la_b = feats
    return (retention_final_state(rk, rv, lg_f),
            retention_final_state(flip_seq(rk), flip_seq(rv), lg_b),
            gla_final_state(gk, gv, la_f),
            gla_final_state(flip_seq(gk), flip_seq(gv), flip_seq(la_b)))


def zero_states(b):
    return (jnp.zeros((b, RET_HEADS, RET_DK, RET_DV), F32),
            jnp.zeros((b, RET_HEADS, RET_DK, RET_DV), F32),
            jnp.zeros((b, GLA_HEADS, GLA_DK, GLA_DV), F32),
            jnp.zeros((b, GLA_HEADS, GLA_DK, GLA_DV), F32))


def peer_ffn(h, w_q, k1, k2, down, up):
    B, L, D = h.shape
    half = PEER_DQ // 2
    blocks = h.reshape(-1, PEER_BLOCK, D)

    def block(xb):
        q = (xb @ w_q).astype(F32).reshape(PEER_BLOCK, PEER_HEADS, 2, half)
        s1 = jnp.einsum("thd,hnd->thn", q[:, :, 0], k1.astype(F32))
        s2 = jnp.einsum("thd,hnd->thn", q[:, :, 1], k2.astype(F32))
        v1, i1 = lax.top_k(s1, PEER_TOPK)
        v2, i2 = lax.top_k(s2, PEER_TOPK)
        cand_s = (v1[..., :, None] + v2[..., None, :]).reshape(PEER_BLOCK, PEER_HEADS, PEER_TOPK * PEER_TOPK)
        cand_i = (i1[..., :, None] * N_KEYS + i2[..., None, :]).reshape(PEER_BLOCK, PEER_HEADS, PEER_TOPK * PEER_TOPK)
        top_s, pos = lax.top_k(cand_s, PEER_TOPK)
        idx = jnp.take_along_axis(cand_i, pos, axis=-1)
        g = jax.nn.softmax(top_s, axis=-1)
        u = down[idx]
        act = jax.nn.gelu(jnp.einsum("thkd,td->thk", u, xb).astype(F32), approximate=False)
        return jnp.einsum("thk,thkd->td", (g * act).astype(xb.dtype), up[idx])

    return lax.map(block, blocks).reshape(B, L, D)


def trunk_layer(x, xc, c, c_ctx, w_mod, b_mod, norm1_w, norm2_w, w_in,
                ret_decay_f, ret_decay_b, gk_up_f, gk_bias_f, gk_up_b, gk_bias_b,
                w_out, peer_w_q, peer_k1, peer_k2, peer_down, peer_up, rope, update_ctx):
    mod = (jax.nn.silu(c) @ w_mod + b_mod)[:, None, :]
    mod_c = (jax.nn.silu(c_ctx) @ w_mod + b_mod)[None, None, :]
    sh1, sc1, g1, sh2, sc2, g2 = jnp.split(mod, N_MOD, axis=-1)
    csh1, csc1, cg1, csh2, csc2, cg2 = jnp.split(mod_c, N_MOD, axis=-1)
    lg_f = jax.nn.log_sigmoid(ret_decay_f.astype(F32))
    lg_b = jax.nn.log_sigmoid(ret_decay_b.astype(F32))

    feats_c = project(modulate(rmsnorm(xc, norm1_w), csh1, csc1),
                      w_in, gk_up_f, gk_bias_f, gk_up_b, gk_bias_b, None)
    if update_ctx:
        yc, ctx_states = bidir_mix(feats_c, lg_f, lg_b, zero_states(xc.shape[0]))
    else:
        ctx_states = context_final_states(feats_c, lg_f, lg_b)

    feats = project(modulate(rmsnorm(x, norm1_w), sh1, sc1),
                    w_in, gk_up_f, gk_bias_f, gk_up_b, gk_bias_b, rope)
    y, _ = bidir_mix(feats, lg_f, lg_b, ctx_states)
    x = x + g1 * (y.astype(x.dtype) @ w_out)
    x = x + g2 * peer_ffn(modulate(rmsnorm(x, norm2_w), sh2, sc2),
                          peer_w_q, peer_k1, peer_k2, peer_down, peer_up)
    if update_ctx:
        xc = xc + cg1 * (yc.astype(xc.dtype) @ w_out)
        xc = xc + cg2 * peer_ffn(modulate(rmsnorm(xc, norm2_w), csh2, csc2),
                                 peer_w_q, peer_k1, peer_k2, peer_down, peer_up)
    return x, xc


def setup_inputs(seed: int = 0) -> dict:
    key = jax.random.key(seed)
    ks = jax.random.split(key, 24)

    def nrm(k, shape, s):
        return jax.random.normal(k, shape, F32) * s

    base_logit = jnp.asarray(np.log(2.0 ** (5 + np.arange(RET_HEADS)) - 1.0), F32)[None, :]
    return {
        "x": nrm(ks[0], (BATCH, SEQ, D_MODEL), 1.0),
        "c": nrm(ks[1], (BATCH, D_MODEL), 1.0),
        "ctx": nrm(ks[2], (BATCH, CTX_LEN, D_MODEL), 1.0),
        "c_ctx": nrm(ks[3], (D_MODEL,), 1.0),
        "w_mod": nrm(ks[4], (DEPTH, D_MODEL, N_MOD * D_MODEL), 0.5 * D_MODEL ** -0.5),
        "b_mod": nrm(ks[5], (DEPTH, N_MOD * D_MODEL), 0.02),
        "norm1_w": 1.0 + nrm(ks[6], (DEPTH, D_MODEL), 0.02),
        "norm2_w": 1.0 + nrm(ks[7], (DEPTH, D_MODEL), 0.02),
        "w_in": nrm(ks[8], (DEPTH, D_MODEL, IN_COLS), D_MODEL ** -0.5),
        "ret_decay_f": base_logit + nrm(ks[9], (DEPTH, RET_HEADS), 0.05),
        "ret_decay_b": base_logit + nrm(ks[10], (DEPTH, RET_HEADS), 0.05),
        "gla_gk_up_f": nrm(ks[11], (DEPTH, GLA_RANK, GLA_HEADS * GLA_DK), GLA_RANK ** -0.5),
        "gla_gk_bias_f": nrm(ks[12], (DEPTH, GLA_HEADS * GLA_DK), 0.1),
        "gla_gk_up_b": nrm(ks[13], (DEPTH, GLA_RANK, GLA_HEADS * GLA_DK), GLA_RANK ** -0.5),
        "gla_gk_bias_b": nrm(ks[14], (DEPTH, GLA_HEADS * GLA_DK), 0.1),
        "w_out": nrm(ks[15], (DEPTH, D_MIX, D_MODEL), D_MIX ** -0.5),
        "peer_w_q": nrm(ks[16], (DEPTH, D_MODEL, PEER_HEADS * PEER_DQ), D_MODEL ** -0.5),
        "peer_k1": nrm(ks[17], (DEPTH, PEER_HEADS, N_KEYS, PEER_DQ // 2), (PEER_DQ // 2) ** -0.5),
        "peer_k2": nrm(ks[18], (DEPTH, PEER_HEADS, N_KEYS, PEER_DQ // 2), (PEER_DQ // 2) ** -0.5),
        "peer_down": nrm(ks[19], (DEPTH, N_EXPERTS, D_MODEL), D_MODEL ** -0.5),
        "peer_up": nrm(ks[20], (DEPTH, N_EXPERTS, D_MODEL), 0.5),
        "norm_f_w": 1.0 + nrm(ks[21], (D_MODEL,), 0.02),
    }


def reference(x, c, ctx, c_ctx, w_mod, b_mod, norm1_w, norm2_w, w_in,
              ret_decay_f, ret_decay_b, gla_gk_up_f, gla_gk_bias_f, gla_gk_up_b, gla_gk_bias_b,
              w_out, peer_w_q, peer_k1, peer_k2, peer_down, peer_up, norm_f_w):
    L = x.shape[1]
    ROWS = L // GRID_W
    rope = axial_rope(ROWS, GRID_W)
    xc = ctx
    for li in range(DEPTH):
        x, xc = trunk_layer(x, xc, c, c_ctx, w_mod[li], b_mod[li], norm1_w[li], norm2_w[li], w_in[li],
                            ret_decay_f[li], ret_decay_b[li], gla_gk_up_f[li], gla_gk_bias_f[li],
                            gla_gk_up_b[li], gla_gk_bias_b[li], w_out[li], peer_w_q[li],
                            peer_k1[li], peer_k2[li], peer_down[li], peer_up[li], rope,
                            li < DEPTH - 1)
    return rmsnorm(x, norm_f_w)
```

```python
import numpy as np
from contextlib import ExitStack
import concourse.bass as bass
import concourse.mybir as mybir
from concourse.bass_utils import run_bass_kernel_spmd

F32 = mybir.dt.float32
BF16 = mybir.dt.bfloat16
I32 = mybir.dt.int32
U32 = mybir.dt.uint32
ALU = mybir.AluOpType
AF = mybir.ActivationFunctionType
AX = mybir.AxisListType

D = 1024
SEQ = 8192
NT = 64
OT = 16
INC = 3616
EPS = 1e-6
MIXBIAS = 0.0
SYNC = 1.5
NEG = -1e30


class Buf:
    __slots__ = ("w", "r")
    registry = []

    def __init__(self):
        self.w = None
        self.r = []
        Buf.registry.append(self)


class Inst:
    __slots__ = ("eng", "fn", "deps", "dma", "need", "sem", "val", "slot", "seq", "cost", "lat")

    def __init__(self, eng, fn, deps, dma, slot):
        self.eng = eng
        self.fn = fn
        self.deps = deps
        self.dma = dma
        self.need = False
        self.sem = None
        self.val = None
        self.slot = slot


class Prog:
    def __init__(self, nc, es):
        self.nc = nc
        self.es = es
        self.insts = []
        self.engs = {"pe": nc.tensor, "dve": nc.vector, "act": nc.scalar,
                     "pool": nc.gpsimd, "sp": nc.sync}

    def op(self, eng, fn, reads=(), writes=(), dma=False, slot=None, cost=0.3, lat=0.0):
        deps = []
        for b in reads:
            if b.w is not None:
                deps.append(b.w)
        for b in writes:
            if b.w is not None:
                deps.append(b.w)
            deps.extend(b.r)
        seen = set()
        d2 = []
        for d in deps:
            if id(d) not in seen:
                seen.add(id(d))
                d2.append(d)
        ins = Inst(eng, fn, d2, dma, slot)
        self.seq = getattr(self, "seq", 0) + 1
        ins.seq = self.seq
        ins.cost = cost
        ins.lat = lat
        for d in d2:
            if dma or d.dma or not (d.eng == "pe" and eng == "pe"):
                d.need = True
        for b in reads:
            b.r.append(ins)
        for b in writes:
            b.w = ins
            b.r = []
        self.insts.append(ins)
        return ins

    def scope_fence(self, bufs):
        if not hasattr(self, "fence"):
            self.fence = {}
        for b in bufs:
            for ins in ([b.w] if b.w is not None else []) + list(b.r):
                key = ("d", ins.slot) if ins.dma else ins.eng
                cur = self.fence.get(key)
                if cur is None or cur.seq < ins.seq:
                    self.fence[key] = ins

    def newbuf(self):
        b = Buf()
        b.r = list(getattr(self, "fence", {}).values())
        return b

    @staticmethod
    def merge(A, B):
        inA = {id(a) for a in A}
        placed = set()
        out = []
        ia = ib = 0
        while ia < len(A) or ib < len(B):
            takeB = ib < len(B) and (ia >= len(A) or ib * len(A) <= ia * len(B))
            if takeB:
                b = B[ib]
                if all((id(d) not in inA) or (id(d) in placed) for d in b.deps):
                    out.append(b)
                    ib += 1
                    continue
            if ia < len(A):
                a = A[ia]
                out.append(a)
                placed.add(id(a))
                ia += 1
            else:
                out.append(B[ib])
                ib += 1
        return out

    @staticmethod
    def schedule(A, B, sync=1.0):
        from collections import deque
        member = {}
        for x in A:
            member[id(x)] = 0
        for x in B:
            member[id(x)] = 1
        q = [dict(), dict()]
        for ci, L in enumerate((A, B)):
            for x in L:
                q[ci].setdefault(x.eng, deque()).append(x)
        done = {}
        efree = {}
        out = []
        n = len(A) + len(B)
        while len(out) < n:
            best = None
            for ci in range(2):
                for e, dq in q[ci].items():
                    if not dq:
                        continue
                    x = dq[0]
                    t = efree.get(e, 0.0)
                    ok = True
                    for d in x.deps:
                        if id(d) in member:
                            dt_ = done.get(id(d))
                            if dt_ is None:
                                ok = False
                                break
                            lt = dt_ + (0.0 if (d.eng == e and e == "pe" and not d.dma) else sync)
                            if lt > t:
                                t = lt
                    if ok:
                        key = t - (bias[ci] if bias else 0.0)
                        if best is None or key < best[0]:
                            best = (key, ci, e, x, t)
            assert best is not None, "scheduler deadlock"
            _, ci, e, x, t = best
            q[ci][e].popleft()
            efree[e] = t + x.cost
            done[id(x)] = t + x.cost + x.lat
            out.append(x)
        return out

    @staticmethod
    def schedule_chains(chains, window=3, sync=1.0, bias=None):
        from collections import deque
        member = set()
        qs = []
        for L in chains:
            qd = {}
            for x in L:
                member.add(id(x))
                qd.setdefault(x.eng, deque()).append(x)
            qs.append(qd)
        remaining = [len(L) for L in chains]
        done = {}
        efree = {}
        out = []
        n = sum(remaining)
        lo = 0
        while len(out) < n:
            while lo < len(chains) and remaining[lo] == 0:
                lo += 1
            best = None
            for ci in range(lo, min(lo + window, len(chains))):
                for e, dq in qs[ci].items():
                    if not dq:
                        continue
                    x = dq[0]
                    t = efree.get(e, 0.0)
                    ok = True
                    for d in x.deps:
                        if id(d) in member:
                            dt_ = done.get(id(d))
                            if dt_ is None:
                                ok = False
                                break
                            lt = dt_ + (0.0 if (d.eng == e and e == "pe" and not d.dma) else sync)
                            if lt > t:
                                t = lt
                    if ok:
                        key = t - (bias[ci] if bias else 0.0)
                        if best is None or key < best[0]:
                            best = (key, ci, e, x, t)
            assert best is not None, "scheduler deadlock"
            _, ci, e, x, t = best
            qs[ci][e].popleft()
            remaining[ci] -= 1
            efree[e] = t + x.cost
            done[id(x)] = t + x.cost + x.lat
            out.append(x)
        return out

    def begin_chains(self):
        self._base = self.insts
        self._chains = []
        self._bias = []
        self._curbias = 0.0
        self.insts = []

    def next_chain(self, bias=0.0):
        if self.insts:
            self._chains.append(self.insts)
            self._bias.append(self._curbias)
        self._curbias = bias
        self.insts = []

    def end_chains(self, window=3):
        self.next_chain()
        self.insts = self._base + Prog.schedule_chains(self._chains, window, sync=SYNC, bias=self._bias)
        self._chains = []

    def flush(self):
        nc = self.nc
        if not hasattr(self, "esem"):
            self.esem = {}
            self.ecnt = {}
            for e in self.engs:
                self.esem[e] = self.es.enter_context(nc.semaphore("s_" + e))
                self.ecnt[e] = 0
            self.slot_sem = {}
            self.slot_cnt = {}
            self.known = {e: {} for e in self.engs}
            self.total = 0
        esem, ecnt, slot_sem, slot_cnt, known = self.esem, self.ecnt, self.slot_sem, self.slot_cnt, self.known
        for b in Buf.registry:
            if b.w is not None:
                b.w.need = True
            for r_ in b.r:
                r_.need = True
        last = {}
        for ins in self.insts:
            if ins.dma:
                ins.need = True
            last[ins.eng] = ins
        for ins in last.values():
            ins.need = True
        self.total += len(self.insts)
        for ins in self.insts:
            eng = self.engs[ins.eng]
            kn = known[ins.eng]
            for d in ins.deps:
                if d.sem is None or (d.eng == "pe" and ins.eng == "pe" and not d.dma):
                    continue
                key = id(d.sem)
                if kn.get(key, 0) < d.val:
                    eng.wait_ge(d.sem, d.val)
                    kn[key] = d.val
            bi = ins.fn()
            if ins.need:
                if ins.dma:
                    s = ins.slot
                    if s not in slot_sem:
                        slot_sem[s] = self.es.enter_context(nc.semaphore("d_%s" % (s,)))
                        slot_cnt[s] = 0
                    slot_cnt[s] += 16
                    bi.then_inc(slot_sem[s], 16)
                    ins.sem = slot_sem[s]
                    ins.val = slot_cnt[s]
                else:
                    ecnt[ins.eng] += 1
                    bi.then_inc(esem[ins.eng], 1)
                    ins.sem = esem[ins.eng]
                    ins.val = ecnt[ins.eng]
        self.insts = []
        self.stats = dict(n=self.total, cnt=dict(ecnt), nslots=len(slot_sem))

    def barrier(self):
        self.flush()
        for e, eng in self.engs.items():
            kn = self.known[e]
            for e2 in self.engs:
                if e2 != e and self.ecnt[e2] > 0 and kn.get(id(self.esem[e2]), 0) < self.ecnt[e2]:
                    eng.wait_ge(self.esem[e2], self.ecnt[e2])
                    kn[id(self.esem[e2])] = self.ecnt[e2]
            for s_, sem in self.slot_sem.items():
                if kn.get(id(sem), 0) < self.slot_cnt[s_]:
                    eng.wait_ge(sem, self.slot_cnt[s_])
                    kn[id(sem)] = self.slot_cnt[s_]


def build(stage="full", n_own=OT, n_state=True):
    nc = bass.Bass("TRN2", target_bir_lowering=False)

    def din(name, shape, dt=F32):
        return nc.dram_tensor(name, list(shape), dt, kind="ExternalInput").ap()

    xb = din("xb", [SEQ, D])
    xo = din("xo", [OT * 128, D])
    ctx = din("ctx", [256, D])
    cT = din("cT", [128, 16])
    w_mod = din("w_mod", [D, 6 * D])
    bmodT = din("bmodT", [128, 48])
    n1T = din("n1T", [128, 8])
    n2row = din("n2row", [1, D])
    nfrow = din("nfrow", [1, D])
    w_in = din("w_in", [D, INC])
    retd = din("retd", [1, 8])
    gkup = din("gkup", [32, 512])
    gkbias = din("gkbias", [1, 512])
    w_out = din("w_out", [D, D])
    w_q = din("w_q", [D, 2048])
    k12T = din("k12T", [128, 2048])
    down = din("down", [16384, D])
    up = din("up", [16384, D])
    cm = din("cm", [128, 8 * 128])
    cs = din("cs", [128, 48])
    ropeb = din("ropeb", [SEQ, 128])
    ropeo = din("ropeo", [OT * 128, 128])
    segsel = din("segsel", [128, 4])
    out_d = nc.dram_tensor("out", [OT * 128, D], F32, kind="ExternalOutput").ap()
    w_in_s = nc.dram_tensor("w_in_s", [D, 4096], BF16, kind="Internal").ap()
    w_q_s = nc.dram_tensor("w_q_s", [D, 2048], BF16, kind="Internal").ap()
    w_out_s = nc.dram_tensor("w_out_s", [D, D], BF16, kind="Internal").ap()
    sbr_s = nc.dram_tensor("sbr_s", [OT, 128, 512], BF16, kind="Internal").ap()
    sbg_s = nc.dram_tensor("sbg_s", [OT, 64, 512], BF16, kind="Internal").ap()
    du_s = nc.dram_tensor("du_s", [16384, 2 * D], BF16, kind="Internal").ap()

    es = ExitStack()
    Buf.registry = []
    P = Prog(nc, es)
    E = P.engs
    p0 = ExitStack()

    PERSIST = [
        ("CM", [128, 8, 128], F32), ("CS", [128, 48], F32), ("IDB", [128, 128], BF16), ("ONESR", [1, 128], F32),
        ("IOTAB", [128, 128], F32), ("LG", [128, 8], F32), ("MASK", [128, 8, 128], F32), ("QDC", [128, 8, 128], F32),
        ("KD", [128, 8], F32), ("CD", [128, 8], F32), ("GKU", [32, 512], BF16), ("GKB", [1, 512], F32),
        ("MODT", [128, 48, 2], F32), ("AB1", [128, 4, 8], F32), ("ROWS", [128, 4, D], F32),
        ("WB", [128, 3, 8, 512], BF16), ("KT", [128, 16, 128], BF16),
        ("SR", [128, 2, 4, 128], F32), ("SG", [64, 2, 4, 128], F32), ("SBR", [128, 2, 4, 128], BF16), ("SBG", [64, 2, 4, 128], BF16),
        ("SEL", [128, 4], F32), ("XT", [128, 2, D], F32), ("ROPE", [128, 2, 128], F32), ("JUNK", [128, D], F32),
        ("ST", [128, 8], F32), ("XH", [128, 1, D], F32), ("HT", [128, 2, 8, 128], BF16), ("KTMP", [128, 4, 128], F32),
        ("KDB", [128, 4, 128], BF16), ("RVB", [128, 512], BF16),
        ("GVB", [128, 512], BF16), ("LRT", [32, 128], BF16), ("LL", [128, 512], F32), ("EG", [128, 256], F32),
        ("KDG", [128, 256], BF16), ("DG", [64, 4], F32),
    ]
    pre = {}
    uniq = [0]
    for (nm, shp, dt_) in PERSIST:
        pre[nm] = es.enter_context(nc.sbuf_tensor(nm, list(shp), dt_))

    def sb(name, shape, dt=F32, scope=None):
        if scope is None:
            t = pre[name]
            assert list(t.shape) == list(shape) and t.dtype == dt, name
            return t
        uniq[0] += 1
        return scope.enter_context(nc.sbuf_tensor("%s_%d" % (name, uniq[0]), list(shape), dt))

    def fsz(ap):
        n_ = 1
        for d_ in ap.shape[1:]:
            n_ *= int(d_)
        return n_

    def dma(q, out, in_, R, W, slot):
        nb_ = fsz(out) * 128 * 4
        return P.op(q, lambda: E[q].dma_start(out=out, in_=in_), R, W, dma=True, slot=slot,
                    cost=(1.0 if q == "pool" else 0.15), lat=2.5 + nb_ / 2.0e5)

    def ecost(eng, out, psum=False):
        n_ = fsz(out)
        if eng == "dve":
            return 0.1 + n_ / (1650.0 if out.dtype == BF16 else 960.0)
        if eng == "act":
            return 0.25 + n_ / 1200.0
        return 0.15 + n_ / 500.0

    def tt(eng, out, a, b, op, R, W):
        return P.op(eng, lambda: E[eng].tensor_tensor(out=out, in0=a, in1=b, op=op), R, W, cost=ecost(eng, out))

    def ts(eng, out, a, s1, op0, R, W, s2=None, op1=None):
        if op1 is None:
            return P.op(eng, lambda: E[eng].tensor_scalar(out=out, in0=a, scalar1=s1, scalar2=None, op0=op0), R, W,
                        cost=ecost(eng, out))
        return P.op(eng, lambda: E[eng].tensor_scalar(out=out, in0=a, scalar1=s1, scalar2=s2, op0=op0, op1=op1), R, W,
                    cost=ecost(eng, out))

    def stt(eng, out, a, s, b, op0, op1, R, W, accum=None):
        if accum is None:
            return P.op(eng, lambda: E[eng].scalar_tensor_tensor(out=out, in0=a, scalar=s, in1=b, op0=op0, op1=op1), R, W,
                        cost=ecost(eng, out))
        return P.op(eng, lambda: E[eng].scalar_tensor_tensor(out=out, in0=a, scalar=s, in1=b, op0=op0, op1=op1, accum_out=accum), R, W,
                    cost=ecost(eng, out))

    def act(out, in_, func, R, W, bias=None, scale=None, accum=None):
        kw = {}
        if bias is not None:
            kw["bias"] = bias
        if scale is not None:
            kw["scale"] = scale
        if accum is not None:
            kw["accum_out"] = accum
        return P.op("act", lambda: nc.scalar.activation(out=out, in_=in_, func=func, **kw), R, W, cost=ecost("act", out))

    def cp(eng, out, in_, R, W):
        if eng == "act":
            return P.op("act", lambda: nc.scalar.copy(out=out, in_=in_), R, W, cost=ecost("act", out))
        return P.op(eng, lambda: E[eng].tensor_copy(out=out, in_=in_), R, W, cost=ecost(eng, out))

    def mm(out, lhsT, rhs, start, stop, R, W):
        c_ = 0.02 + fsz(rhs) / 1300.0 * (4.0 if rhs.dtype == F32 else 1.0)
        return P.op("pe", lambda: nc.tensor.matmul(out, lhsT=lhsT, rhs=rhs, start=start, stop=stop), R, W, cost=c_)

    def tr(out, in_, ident, R, W):
        c_ = 0.02 + 128 / 1300.0 * (4.0 if in_.dtype == F32 else 1.0)
        return P.op("pe", lambda: nc.tensor.transpose(out=out, in_=in_, identity=ident), R, W, cost=c_)

    def memset(eng, ap, v, W):
        return P.op(eng, lambda: E[eng].memset(ap, v), (), W)

    def red(eng, out, in_, op, R, W):
        return P.op(eng, lambda: E[eng].tensor_reduce(out=out, in_=in_, axis=AX.X, op=op), R, W, cost=ecost(eng, in_))

    psf = [es.enter_context(nc.psum_tensor("ps%d" % i, [128, 512], F32)) for i in range(7)]
    psb = es.enter_context(nc.psum_tensor("psb", [128, 1024], BF16))
    bps = [Buf() for _ in range(7)]
    bpsb = Buf()

    CM = sb("CM", [128, 8, 128])
    CS = sb("CS", [128, 48])
    bC = Buf()
    dma("sp", CM[:].rearrange("p a b -> p (a b)"), cm, (), [bC], "c0")
    dma("sp", CS[:], cs, (), [bC], "c0")
    IDENT = CM[:, 0, :]
    SLE = CM[:, 1, :]
    SGE = CM[:, 2, :]
    RELF = CM[:, 3, :]
    RELB = CM[:, 4, :]
    SGT = CM[:, 5, :]
    SLT = CM[:, 6, :]
    IOTAF = CM[:, 7, :]
    COLF = CS[:, 0:4]
    COLB = CS[:, 4:8]
    IOTA16 = CS[:, 8:24]
    ONESC = CS[:, 24:25]
    THR16 = CS[:, 32:48]
    IDB = sb("IDB", [128, 128], BF16)
    bIDB = Buf()
    cp("dve", IDB[:], IDENT, [bC], [bIDB])
    ONESR = sb("ONESR", [1, 128])
    bONESR = Buf()
    memset("dve", ONESR[:], 1.0, [bONESR])
    IOTAB = sb("IOTAB", [128, 128])
    ts("dve", IOTAB[:], IOTAF, -1.0, ALU.mult, [bC], [bC], 129.0, ALU.add)

    RD = sb("RD", [128, 8], scope=p0)
    LG = sb("LG", [128, 8])
    bLG = Buf()
    dma("act", RD[:], retd.partition_broadcast(128), (), [bLG], "c1")
    act(LG[:], RD[:], AF.Exp, [bLG], [bLG], scale=-1.0)
    act(RD[:], LG[:], AF.Ln, [bLG], [bLG], bias=1.0)
    ts("dve", LG[:], RD[:], -1.0, ALU.mult, [bLG], [bLG])
    MASK = sb("MASK", [128, 8, 128])
    QDC = sb("QDC", [128, 8, 128])
    KD = sb("KD", [128, 8])
    CD = sb("CD", [128, 8])
    bDC = Buf()
    RSC = 128.0 ** -0.5
    for d_ in range(2):
        rel = RELF if d_ == 0 else RELB
        tri = SLE if d_ == 0 else SGE
        iot = IOTAF if d_ == 0 else IOTAB[:]
        for h in range(4):
            c = d_ * 4 + h
            act(MASK[:, c, :], rel, AF.Exp, [bC, bLG], [bDC], scale=LG[:, c:c + 1])
            stt("dve", MASK[:, c, :], MASK[:, c, :], RSC, tri, ALU.mult, ALU.mult, [bDC, bC], [bDC])
            act(QDC[:, c, :], iot, AF.Exp, [bC, bLG], [bDC], scale=LG[:, c:c + 1])
    tt("dve", KD[:, 0:4], COLF, LG[:, 0:4], ALU.mult, [bC, bLG], [bDC])
    tt("dve", KD[:, 4:8], COLB, LG[:, 4:8], ALU.mult, [bC, bLG], [bDC])
    act(KD[:], KD[:], AF.Exp, [bDC], [bDC])
    ts("dve", KD[:], KD[:], RSC, ALU.mult, [bDC], [bDC])
    act(CD[:], LG[:], AF.Exp, [bLG], [bDC], scale=128.0)

    STG = sb("STG", [128, 8, 512], scope=p0)
    bSTG = Buf()
    STGB = sb("STGB", [128, 8, 512], F32, scope=p0)
    bSTGB = Buf()
    GKU = sb("GKU", [32, 512], BF16)
    GKB = sb("GKB", [1, 512])
    bGK = Buf()
    dma("act", STG[0:32, 0, :], gkup, (), [bSTG], "stg")
    cp("dve", GKU[:], STG[0:32, 0, :], [bSTG], [bGK])
    dma("act", GKB[:], gkbias, (), [bGK], "c2")

    SC = sb("SC", [128, 16], scope=p0)
    bSC = Buf()
    dma("sp", SC[:], cT, (), [bSC], "c3")
    act(SC[:], SC[:], AF.Silu, [bSC], [bSC])
    MODT = sb("MODT", [128, 48, 2])
    bMOD = Buf()
    BM = sb("BM", [128, 48], scope=p0)
    dma("sp", BM[:], bmodT, (), [bMOD], "c4")
    SC3 = SC[:].rearrange("p (a k) -> p a k", a=2)
    for g in range(12):
        SGt, bSGt = (STG, bSTG) if g % 2 == 0 else (STGB, bSTGB)
        dma("sp" if g % 2 == 0 else "act", SGt[:], w_mod[:, g * 512:(g + 1) * 512].rearrange("(k p) n -> p k n", p=128),
            (), [bSGt], "stg%d" % (g % 2))
        pb = g % 2
        for c4 in range(4):
            for kk in range(8):
                mm(psf[pb][:, c4 * 2:c4 * 2 + 2], SGt[:, kk, c4 * 128:(c4 + 1) * 128], SC3[:, :, kk],
                   kk == 0, kk == 7, [bSGt, bSC], [bps[pb]])
        cp("dve", MODT[:, g * 4:(g + 1) * 4, :].rearrange("p a b -> p (a b)"), psf[pb][:, 0:8], [bps[pb]], [bMOD])
    tt("dve", MODT[:], MODT[:], BM[:].unsqueeze(2).to_broadcast([128, 48, 2]), ALU.add, [bMOD], [bMOD])
    N1 = sb("N1", [128, 8], scope=p0)
    dma("sp", N1[:], n1T, (), [bMOD], "c5")
    AB1 = sb("AB1", [128, 4, 8])
    bAB = Buf()
    for v in range(2):
        stt("dve", AB1[:, 2 * v, :], MODT[:, 8:16, v], 1.0, N1[:], ALU.add, ALU.mult, [bMOD], [bAB])
        cp("dve", AB1[:, 2 * v + 1, :], MODT[:, 0:8, v], [bMOD], [bAB])
    ROWS = sb("ROWS", [128, 4, D])
    bROWS = Buf()
    G1R = sb("G1R", [128, D], scope=p0)
    bG1R = Buf()
    CREP = sb("CREP", [128, 2, 128], scope=p0)
    bCREP = [Buf(), Buf()]
    N2R = sb("N2R", [128, D], scope=p0)
    dma("act", N2R[:], n2row.partition_broadcast(128), (), [bROWS], "c6")
    dma("act", ROWS[:, 3, :], nfrow.partition_broadcast(128), (), [bROWS], "c6")
    cnt = 0
    for (j, dst) in ((2, G1R[:]), (3, ROWS[:, 1, :]), (4, ROWS[:, 0, :]), (5, ROWS[:, 2, :])):
        for k in range(8):
            s = cnt % 2
            cnt += 1
            cp("dve", CREP[:, s, :], MODT[:, j * 8 + k, 0:1].to_broadcast([128, 128]), [bMOD], [bCREP[s]])
            mm(psf[2 + s][:, 0:128], CREP[:, s, :], IDENT, True, True, [bCREP[s], bC], [bps[2 + s]])
            cp("act", dst[:, k * 128:(k + 1) * 128], psf[2 + s][:, 0:128], [bps[2 + s]], [bG1R if j == 2 else bROWS])
    stt("dve", ROWS[:, 0, :], ROWS[:, 0, :], 1.0, N2R[:], ALU.add, ALU.mult, [bROWS], [bROWS])
    A2 = ROWS[:, 0, :]
    B2 = ROWS[:, 1, :]
    G2 = ROWS[:, 2, :]
    NF = ROWS[:, 3, :]

    WB = sb("WB", [128, 3, 8, 512], BF16)
    bWB = [Buf(), Buf(), Buf()]
    bWSall = []
    nconv = 0

    def conv(src, dst, ncols, fold=None):
        nonlocal nconv
        s = nconv % 2
        nconv += 1
        SGt, bSGt = (STG, bSTG) if s == 0 else (STGB, bSTGB)
        dma("sp", SGt[:, :, 0:ncols], src.rearrange("(k p) n -> p k n", p=128), (), [bSGt], "stg%d" % s)
        for kk in range(8):
            eng = "dve" if kk % 2 == 0 else "pool"
            if fold is None:
                cp(eng, WB[:, s, kk, 0:ncols], SGt[:, kk, 0:ncols], [bSGt], [bWB[s]])
            else:
                tt(eng, WB[:, s, kk, 0:ncols], SGt[:, kk, 0:ncols], fold, ALU.mult, [bSGt, bG1R], [bWB[s]])
        bw_ = Buf()
        bWSall.append(bw_)
        dma("act", dst.rearrange("(k p) n -> p k n", p=128), WB[:, s, :, 0:ncols], [bWB[s]], [bw_], "ws%d" % s)

    for g in range(8):
        n = 512 if g < 7 else 32
        conv(w_in[:, g * 512:g * 512 + n], w_in_s[:, g * 512:g * 512 + n], n)
    for g in range(4):
        conv(w_q[:, g * 512:(g + 1) * 512], w_q_s[:, g * 512:(g + 1) * 512], 512)
    for g in range(2):
        conv(w_out[:, g * 512:(g + 1) * 512], w_out_s[:, g * 512:(g + 1) * 512], 512, fold=G1R[:, g * 512:(g + 1) * 512])

    KT = sb("KT", [128, 16, 128], BF16)
    bKT = Buf()
    dma("sp", STG[:, 0:4, :].rearrange("p a b -> p (a b)"), k12T, (), [bSTG], "stg")
    cp("dve", KT[:].rearrange("p a b -> p (a b)"), STG[:, 0:4, :].rearrange("p a b -> p (a b)"), [bSTG], [bKT])
    P.barrier()
    print("sbuf after phase0", nc.sbuf_bytes_remaining)
    p0.close()
    p12 = ExitStack()
    WKV = sb("WKV", [128, 8, 1824], BF16, scope=p12)
    bWKV = Buf()
    for (o, c0, n) in ((0, 512, 512), (512, 1024, 512), (1024, 2304, 256), (1280, 2560, 512), (1792, 3584, 32)):
        dma("sp", WKV[:, :, o:o + n], w_in_s[:, c0:c0 + n].rearrange("(k p) n -> p k n", p=128), bWSall, [bWKV], "wkv")

    TF = sb("TF", [128, 2, 2048], F32, scope=p12)
    TBF = sb("TBF", [128, 2, 2048], BF16, scope=p12)
    bTF = [Buf(), Buf()]
    bTBF = [Buf(), Buf()]
    bTAB = Buf()
    tconv = [0]

    def table_chunk():
        i = tconv[0]
        if i >= 128:
            return
        tconv[0] += 1
        src = down if i < 64 else up
        c0 = 0 if i < 64 else D
        c = i % 64
        s_ = i % 2
        sv = src[c * 256:(c + 1) * 256, :].rearrange("(p r) d -> p (r d)", p=128)
        dv = du_s[c * 256:(c + 1) * 256, c0:c0 + D].rearrange("(p r) d -> p r d", p=128)
        dma("pool", TF[:, s_, :], sv, (), [bTF[s_]], "tf%d" % s_)
        cp("pool", TBF[:, s_, :], TF[:, s_, :], [bTF[s_]], [bTBF[s_]])
        dma("pool", dv, TBF[:, s_, :].rearrange("p (r d) -> p r d", r=2), [bTBF[s_]], [bTAB], "tb%d" % s_)

    SR = sb("SR", [128, 2, 4, 128])
    SG = sb("SG", [64, 2, 4, 128])
    bS = [[Buf() for _ in range(8)] for _ in range(2)]
    SI_R = sb("SI_R", [128, 2, 4, 128], F32, scope=p12)
    SI_G = sb("SI_G", [64, 2, 4, 128], F32, scope=p12)
    bSI = [Buf(), Buf()]
    memset("pool", SI_R[:].rearrange("p a b c -> p (a b c)"), 0.0, [bSI[0]])
    memset("pool", SI_G[:].rearrange("p a b c -> p (a b c)"), 0.0, [bSI[1]])
    memset("pool", SR[:].rearrange("p a b c -> p (a b c)"), 0.0, [bS[d_][i] for d_ in range(2) for i in range(4)])
    memset("pool", SG[:].rearrange("p a b c -> p (a b c)"), 0.0, [bS[d_][4 + i] for d_ in range(2) for i in range(4)])
    SBR = sb("SBR", [128, 2, 4, 128], BF16)
    SBG = sb("SBG", [64, 2, 4, 128], BF16)
    bSBT = [Buf(), Buf()]
    bSB = [Buf() for _ in range(OT)]
    SEL = sb("SEL", [128, 4])
    bSEL = Buf()
    dma("sp", SEL[:], segsel, (), [bSEL], "c7")

    XT = sb("XT", [128, 2, D])
    bXT = [Buf(), Buf()]
    ROPE = sb("ROPE", [128, 2, 128])
    bRP = [Buf(), Buf()]
    JUNK = sb("JUNK", [128, D])
    bJUNK = Buf()
    ST = sb("ST", [128, 8])
    bST = Buf()
    XH2 = sb("XH", [128, 1, D])
    bXH2 = [Buf(), Buf()]
    bXH2[1] = bXH2[0]
    HT2 = sb("HT", [128, 2, 8, 128], BF16)
    bHT2 = [Buf(), Buf()]
    KTMP = sb("KTMP", [128, 4, 128])
    RTMP = None
    bKTMP = Buf()
    KDB = sb("KDB", [128, 4, 128], BF16)
    bKDB = Buf()
    RVB = sb("RVB", [128, 512], BF16)
    bRVB = Buf()
    GVB = sb("GVB", [128, 512], BF16)
    bGVB = Buf()
    LRT = sb("LRT", [32, 128], BF16)
    bLRT = Buf()
    LL = sb("LL", [128, 512])
    bLL = Buf()
    EG = sb("EG", [128, 256])
    bEG = Buf()
    KDG = sb("KDG", [128, 256], BF16)
    bKDG = Buf()
    DG = sb("DG", [64, 4])
    bDG = Buf()

    vis = [0]

    def norm_and_transpose(src_ap, ab, rope_ap):
        s = vis[0] % 2
        vis[0] += 1
        XH = XH2[:, 0, :]
        bXH = bXH2[s]
        HT = HT2[:, s]
        bHT = bHT2[s]
        dma("sp", XT[:, s, :], src_ap, (), [bXT[s]], "xt%d" % s)
        if rope_ap is not None:
            dma("act", ROPE[:, s, :], rope_ap, (), [bRP[s]], "rp%d" % s)
        act(XH, XT[:, s, :], AF.Square, [bXT[s]], [bXH, bST], accum=ST[:, 0:1])
        ts("dve", ST[:, 1:2], ST[:, 0:1], 1.0 / D, ALU.mult, [bST], [bST], EPS, ALU.add)
        act(ST[:, 2:3], ST[:, 1:2], AF.Sqrt, [bST], [bST])
        P.op("dve", lambda: nc.vector.reciprocal(out=ST[:, 3:4], in_=ST[:, 2:3]), [bST], [bST])
        ts("dve", XH, XT[:, s, :], ST[:, 3:4], ALU.mult, [bXT[s], bST], [bXH])
        for half in range(2):
            pb = half
            for k4 in range(4):
                k = half * 4 + k4
                tr(psf[pb][:, k4 * 128:(k4 + 1) * 128], XH[:, k * 128:(k + 1) * 128], IDENT, [bXH, bC], [bps[pb]])
            for k4 in range(4):
                k = half * 4 + k4
                if k4 % 2 == 0:
                    act(HT[:, k, :], psf[pb][:, k4 * 128:(k4 + 1) * 128], AF.Identity, [bps[pb], bAB], [bHT],
                        bias=AB1[:, ab + 1, k:k + 1], scale=AB1[:, ab, k:k + 1])
                else:
                    ts("dve", HT[:, k, :], psf[pb][:, k4 * 128:(k4 + 1) * 128], AB1[:, ab, k:k + 1], ALU.mult,
                       [bps[pb], bAB], [bHT], AB1[:, ab + 1, k:k + 1], ALU.add)
        return s, HT, bHT

    def rope_apply(dst, src_ps, s, Rsrc, Wdst):
        x1 = src_ps.rearrange("p (h a b) -> p h a b", h=4, a=2)[:, :, 0, :]
        x2 = src_ps.rearrange("p (h a b) -> p h a b", h=4, a=2)[:, :, 1, :]
        RT = JUNK[:].rearrange("p (a h e) -> p a h e", a=4, h=4)
        cos = ROPE[:, s, 0:64].unsqueeze(1).to_broadcast([128, 4, 64])
        sin = ROPE[:, s, 64:128].unsqueeze(1).to_broadcast([128, 4, 64])
        tt("dve", RT[:, 0], x1, cos, ALU.mult, Rsrc + [bRP[s]], [bJUNK])
        tt("dve", RT[:, 1], x2, sin, ALU.mult, Rsrc + [bRP[s]], [bJUNK])
        tt("dve", RT[:, 2], x2, cos, ALU.mult, Rsrc + [bRP[s]], [bJUNK])
        tt("dve", RT[:, 3], x1, sin, ALU.mult, Rsrc + [bRP[s]], [bJUNK])
        tt("dve", dst[:, :, 0:64], RT[:, 0], RT[:, 1], ALU.subtract, [bJUNK], Wdst)
        tt("dve", dst[:, :, 64:128], RT[:, 2], RT[:, 3], ALU.add, [bJUNK], Wdst)

    def gate_L(d_list, gb=5):
        for d_ in d_list:
            cs_ = slice(d_ * 256, (d_ + 1) * 256)
            mm(psf[gb][:, cs_], LRT[:, :], GKU[:, cs_], True, False, [bLRT, bGK], [bps[gb]])
            mm(psf[gb][:, cs_], ONESR[:, :], GKB[:, cs_], False, True, [bONESR, bGK], [bps[gb]])
            act(LL[:, cs_], psf[gb][:, cs_], AF.Exp, [bps[gb]], [bLL], scale=-1.0)
            act(LL[:, cs_], LL[:, cs_], AF.Ln, [bLL], [bLL], bias=1.0)

    def gla_kdecay(d_, gk_ps, Rgk, kb=6):
        cs_ = slice(d_ * 256, (d_ + 1) * 256)
        m = SGT if d_ == 0 else SLT
        mm(psf[kb][:, 0:256], m, LL[:, cs_], True, True, [bC, bLL], [bps[kb]])
        act(EG[:], psf[kb][:, 0:256], AF.Exp, [bps[kb]], [bEG], scale=-1.0 / 16)
        tt("dve", KDG[:], gk_ps, EG[:], ALU.mult, Rgk + [bEG], [bKDG])
        for h in range(4):
            mm(psf[kb][0:64, 256 + h:257 + h], LL[:, d_ * 256 + h * 64:d_ * 256 + (h + 1) * 64], ONESC,
               True, True, [bLL, bC], [bps[kb]])
        act(DG[:], psf[kb][0:64, 256:260], AF.Exp, [bps[kb]], [bDG], scale=-1.0 / 16)

    def state_update(d_, pbA, pbB):
        for h in range(4):
            mm(psf[pbA][:, h * 128:(h + 1) * 128], KDB[:, h, :], RVB[:, h * 128:(h + 1) * 128], True, True,
               [bKDB, bRVB], [bps[pbA]])
        for h in range(4):
            mm(psf[pbB][0:64, h * 128:(h + 1) * 128], KDG[:, h * 64:(h + 1) * 64], GVB[:, h * 128:(h + 1) * 128],
               True, True, [bKDG, bGVB], [bps[pbB]])
        for h in range(4):
            stt("dve", SR[:, d_, h, :], SR[:, d_, h, :], CD[:, d_ * 4 + h:d_ * 4 + h + 1],
                psf[pbA][:, h * 128:(h + 1) * 128], ALU.mult, ALU.add, [bps[pbA], bDC], [bS[d_][h]])
        for h in range(4):
            stt("dve", SG[:, d_, h, :], SG[:, d_, h, :], DG[:, h:h + 1],
                psf[pbB][0:64, h * 128:(h + 1) * 128], ALU.mult, ALU.add, [bps[pbB], bDG], [bS[d_][4 + h]])

    def state_visit(src_ap, d_, is_ctx, rope_ap):
        P.next_chain()
        table_chunk()
        s, HT, bHT = norm_and_transpose(src_ap, 2 if is_ctx else 0, rope_ap)
        for k in range(8):
            mm(psf[2][:, :], HT[:, k, :], WKV[:, k, 0:512], k == 0, k == 7, [bHT, bWKV], [bps[2]])
        for k in range(8):
            mm(psf[3][:, :], HT[:, k, :], WKV[:, k, 512:1024], k == 0, k == 7, [bHT, bWKV], [bps[3]])
        for k in range(8):
            mm(psf[4][:, 0:256], HT[:, k, :], WKV[:, k, 1024:1280], k == 0, k == 7, [bHT, bWKV], [bps[4]])
        for k in range(8):
            mm(psf[4][0:32, 256:384], WKV[:, k, 1792:1824], HT[:, k, :], k == 0, k == 7, [bHT, bWKV], [bps[4]])
        cp("act", LRT[:], psf[4][0:32, 256:384], [bps[4]], [bLRT])
        kdv = KD[:, d_ * 4:(d_ + 1) * 4].unsqueeze(2).to_broadcast([128, 4, 128])
        if is_ctx:
            tt("dve", KDB[:], psf[2][:, :].rearrange("p (h e) -> p h e", h=4), kdv, ALU.mult,
               [bps[2], bDC], [bKDB])
        else:
            rope_apply(KTMP, psf[2][:, :], s, [bps[2]], [bKTMP])
            tt("dve", KDB[:], KTMP[:], kdv, ALU.mult, [bKTMP, bDC], [bKDB])
        cp("act", RVB[:], psf[3][:, :], [bps[3]], [bRVB])
        for k in range(8):
            mm(psf[3][:, :], HT[:, k, :], WKV[:, k, 1280:1792], k == 0, k == 7, [bHT, bWKV], [bps[3]])
        cp("act", GVB[:], psf[3][:, :], [bps[3]], [bGVB])
        gate_L([d_])
        gla_kdecay(d_, psf[4][:, 0:256], [bps[4]])
        state_update(d_, 5, 6)

    def snapshot(d_, sidx):
        for h in range(4):
            stt("dve", SI_R[:, d_, h, :], SR[:, d_, h, :], SEL[:, sidx:sidx + 1], SI_R[:, d_, h, :],
                ALU.mult, ALU.add, [bS[d_][h], bSEL], [bSI[0]])
            stt("dve", SI_G[:, d_, h, :], SG[:, d_, h, :], SEL[0:64, sidx:sidx + 1], SI_G[:, d_, h, :],
                ALU.mult, ALU.add, [bS[d_][4 + h], bSEL], [bSI[1]])

    def xb_tile(i):
        return xb[i * 128:(i + 1) * 128, :]

    P.begin_chains()
    if n_state:
        state_visit(ctx[0:128, :], 0, True, None)
        state_visit(ctx[128:256, :], 0, True, None)
        snapshot(0, 0)
        for i in range(48):
            state_visit(xb_tile(i), 0, False, ropeb[i * 128:(i + 1) * 128, :])
            if i % 16 == 15:
                snapshot(0, i // 16 + 1)
        state_visit(ctx[128:256, :], 1, True, None)
        state_visit(ctx[0:128, :], 1, True, None)
        snapshot(1, 3)
        for i in range(63, 15, -1):
            state_visit(xb_tile(i), 1, False, ropeb[i * 128:(i + 1) * 128, :])
            if i % 16 == 0:
                snapshot(1, i // 16 - 1)
    for h in range(4):
        cp("dve", SR[:, 1, h, :], SI_R[:, 1, h, :], [bSI[0]], [bS[1][h]])
        cp("dve", SG[:, 1, h, :], SI_G[:, 1, h, :], [bSI[1]], [bS[1][4 + h]])
    for c in range(n_own - 1, -1, -1):
        for h in range(4):
            cp("pool", SBR[:, c % 2, h, :], SR[:, 1, h, :], [bS[1][h]], [bSBT[c % 2]])
            cp("pool", SBG[:, c % 2, h, :], SG[:, 1, h, :], [bS[1][4 + h]], [bSBT[c % 2]])
        dma("sp", sbr_s[c], SBR[:, c % 2].rearrange("p a b -> p (a b)"), [bSBT[c % 2]], [bSB[c]], "sbw%d" % (c % 2))
        dma("sp", sbg_s[c], SBG[:, c % 2].rearrange("p a b -> p (a b)"), [bSBT[c % 2]], [bSB[c]], "sbw%d" % (c % 2))
        if c > 0:
            state_visit(xo[c * 128:(c + 1) * 128, :], 1, False, ropeo[c * 128:(c + 1) * 128, :])
    for h in range(4):
        cp("dve", SR[:, 0, h, :], SI_R[:, 0, h, :], [bSI[0]], [bS[0][h]])
        cp("dve", SG[:, 0, h, :], SI_G[:, 0, h, :], [bSI[1]], [bS[0][4 + h]])

    while tconv[0] < 128:
        table_chunk()
    P.end_chains(4)
    P.barrier()
    print("sbuf after pass2", nc.sbuf_bytes_remaining)
    p12.close()
    p3 = ExitStack()
    NG = 12
    UV = sb("UV", [128, NG, 2 * D], BF16, p3)
    bUV = [Buf() for _ in range(NG)]
    DGS = sb("DGS", [128, 4, 128], BF16, p3)
    bDGS = [Buf() for _ in range(4)]
    JB = sb("JB", [128, D], BF16, p3)
    bJB = Buf()
    ACTV = sb("ACTV", [128, 128], F32, p3)
    bA = [Buf() for _ in range(128)]
    bCf = [Buf() for _ in range(64)]
    COEF = sb("COEF", [128, 128], F32, p3)
    ACC = sb("ACC", [128, D], F32, p3)
    bACC = Buf()
    ST2 = sb("ST2", [128, 8], F32, p3)
    bST2 = Buf()
    X1D = sb("X1D", [128, 2, D], F32, p3)
    bX1 = [Buf(), Buf()]
    H2BD = sb("H2BD", [128, 2, D], BF16, p3)
    bH2B = [Buf(), Buf()]
    IDXD = sb("IDXD", [128, 2, 128], I32, p3)
    bIDX = [Buf(), Buf()]
    GWD = sb("GWD", [128, 2, 128], F32, p3)
    bGW = [Buf(), Buf()]
    finals = []
    wcnt = [0]

    def wload(src, n):
        s = wcnt[0] % 3
        wcnt[0] += 1
        dma("sp", WB[:, s, :, 0:n], src.rearrange("(k p) n -> p k n", p=128), bWSall, [bWB[s]], "wb%d" % s)
        return s

    def mix(c):
        pc = c % 2
        mx = ExitStack()
        bl = []

        def nb():
            b_ = P.newbuf()
            bl.append(b_)
            return b_
        QR = sb("QR", [128, 4, 128], BF16, mx)
        KR = sb("KR", [128, 4, 128], BF16, mx)
        bQR = nb()
        bKR = nb()
        GQK = sb("GQK", [128, 512], BF16, mx)
        bGQK = nb()
        SGATE = sb("SGATE", [128, D], F32, mx)
        bSGATE = nb()
        QKT = sb("QKT", [128, 8, 128], BF16, mx)
        bQKT = nb()
        GT = sb("GT", [64, 8, 128], BF16, mx)
        bGT = nb()
        EQK = sb("EQK", [64, 2, 4, 128], F32, mx)
        bEQK = nb()
        QKTT = sb("QKTT", [64, 2, 2, 4, 128], BF16, mx)
        bQKTT = [nb(), nb()]
        SM = sb("SM", [128, 4, 128], BF16, mx)
        bSM = [nb() for _ in range(4)]
        QD = sb("QD", [128, 4, 128], BF16, mx)
        bQD = [nb() for _ in range(4)]
        SBF = sb("SBF", [128, 8, 128], BF16, mx)
        bSBF = nb()
        YB = sb("YB", [128, D], BF16, mx)
        bYB = nb()
        YT = sb("YT", [128, 8, 128], BF16, mx)
        bYT = nb()
        SBRc = sb("SBRc", [128, 4, 128], BF16, mx)
        SBGc = sb("SBGc", [64, 4, 128], BF16, mx)
        bSBc = nb()
        dma("sp", SBRc[:].rearrange("p a b -> p (a b)"), sbr_s[c], [bSB[c]], [bSBc], "sbl")
        dma("sp", SBGc[:].rearrange("p a b -> p (a b)"), sbg_s[c], [bSB[c]], [bSBc], "sbl")
        s, HT, bHT = norm_and_transpose(xo[c * 128:(c + 1) * 128, :], 0, ropeo[c * 128:(c + 1) * 128, :])
        for h in range(4):
            cp("act", SBF[:, h, :], SR[:, 0, h, :], [bS[0][h]], [bSBF])
            cp("act", SBF[0:64, 4 + h, :], SG[:, 0, h, :], [bS[0][4 + h]], [bSBF])
        for g in range(8):
            n = 512 if g < 7 else 32
            ws = wload(w_in_s[:, g * 512:g * 512 + n], n)
            pb = 2 + g % 2
            if g < 7:
                for k in range(8):
                    mm(psf[pb][:, :], HT[:, k, :], WB[:, ws, k, :], k == 0, k == 7, [bHT, bWB[ws]], [bps[pb]])
            else:
                for k in range(8):
                    mm(psf[pb][0:32, 0:128], WB[:, ws, k, 0:32], HT[:, k, :], k == 0, k == 7, [bHT, bWB[ws]], [bps[pb]])
            pv = psf[pb][:, :]
            if g == 0:
                rope_apply(KTMP, pv, s, [bps[pb]], [bKTMP])
                cp("act", QR[:], KTMP[:], [bKTMP], [bQR])
            elif g == 1:
                rope_apply(KTMP, pv, s, [bps[pb]], [bKTMP])
                cp("act", KR[:], KTMP[:], [bKTMP], [bKR])
                tt("dve", KDB[:], KTMP[:], KD[:, 0:4].unsqueeze(2).to_broadcast([128, 4, 128]), ALU.mult,
                   [bKTMP, bDC], [bKDB])
            elif g == 2:
                cp("act", RVB[:], pv, [bps[pb]], [bRVB])
            elif g == 3:
                act(SGATE[:, 0:512], pv, AF.Silu, [bps[pb]], [bSGATE])
            elif g == 4:
                cp("act", GQK[:], pv, [bps[pb]], [bGQK])
            elif g == 5:
                cp("act", GVB[:], pv, [bps[pb]], [bGVB])
            elif g == 6:
                act(SGATE[:, 512:1024], pv, AF.Silu, [bps[pb]], [bSGATE])
            else:
                cp("act", LRT[:], psf[pb][0:32, 0:128], [bps[pb]], [bLRT])
        for h in range(4):
            tr(psb[:, h * 128:(h + 1) * 128], QR[:, h, :], IDB[:], [bQR, bIDB], [bpsb])
            tr(psb[:, (4 + h) * 128:(5 + h) * 128], KR[:, h, :], IDB[:], [bKR, bIDB], [bpsb])
        cp("act", QKT[:].rearrange("p a b -> p (a b)"), psb[:, :], [bpsb], [bQKT])
        for h in range(8):
            tr(psb[0:64, h * 128:(h + 1) * 128], GQK[:, h * 64:(h + 1) * 64], IDB[:], [bGQK, bIDB], [bpsb])
        cp("act", GT[:].rearrange("p a b -> p (a b)"), psb[0:64, :], [bpsb], [bGT])
        gate_L([0, 1], 4)
        for d_ in range(2):
            m = SLE if d_ == 0 else SGE
            pb = d_
            for h in range(4):
                mm(psf[pb][0:64, h * 128:(h + 1) * 128], LL[:, d_ * 256 + h * 64:d_ * 256 + (h + 1) * 64], m,
                   True, True, [bLL, bC], [bps[pb]])
            act(EQK[:, 0].rearrange("p a b -> p (a b)"), psf[pb][0:64, :], AF.Exp, [bps[pb]], [bEQK],
                scale=-1.0 / 16)
            act(EQK[:, 1].rearrange("p a b -> p (a b)"), psf[pb][0:64, :], AF.Exp, [bps[pb]], [bEQK],
                scale=1.0 / 16)
            stt("dve", QKTT[:, d_, 0].rearrange("p a b -> p (a b)"), GT[:, 0:4, :].rearrange("p a b -> p (a b)"),
                0.125, EQK[:, 0].rearrange("p a b -> p (a b)"), ALU.mult, ALU.mult, [bGT, bEQK], [bQKTT[d_]])
            tt("dve", QKTT[:, d_, 1].rearrange("p a b -> p (a b)"), GT[:, 4:8, :].rearrange("p a b -> p (a b)"),
               EQK[:, 1].rearrange("p a b -> p (a b)"), ALU.mult, [bGT, bEQK], [bQKTT[d_]])
        gla_kdecay(0, GQK[:, 256:512], [bGQK], 4)
        rot = 0
        for h in range(4):
            oreg = psf[2][:, h * 128:(h + 1) * 128]
            for d_ in range(2):
                c_ = d_ * 4 + h
                r = rot % 4
                rot += 1
                scp = psf[4][:, r * 128:(r + 1) * 128]
                mm(scp, QKT[:, 4 + h, :], QKT[:, h, :], True, True, [bQKT], [bps[4]])
                tt("dve", SM[:, r, :], scp, MASK[:, c_, :], ALU.mult, [bps[4], bDC], [bSM[r]])
                mm(oreg, SM[:, r, :], RVB[:, h * 128:(h + 1) * 128], d_ == 0, False, [bSM[r], bRVB], [bps[2]])
                tt("dve", QD[:, r, :], QKT[:, h, :], QDC[:, c_, :], ALU.mult, [bQKT, bDC], [bQD[r]])
                if d_ == 0:
                    mm(oreg, QD[:, r, :], SBF[:, h, :], False, False, [bQD[r], bSBF], [bps[2]])
                else:
                    mm(oreg, QD[:, r, :], SBRc[:, h, :], False, True, [bQD[r], bSBc], [bps[2]])
        for h in range(4):
            oreg = psf[3][:, h * 128:(h + 1) * 128]
            for d_ in range(2):
                r = rot % 4
                rot += 1
                scp = psf[4][:, r * 128:(r + 1) * 128]
                mm(scp, QKTT[:, d_, 1, h, :], QKTT[:, d_, 0, h, :], True, True, [bQKTT[d_]], [bps[4]])
                tt("dve", SM[:, r, :], scp, SLE if d_ == 0 else SGE, ALU.mult, [bps[4], bC], [bSM[r]])
                mm(oreg, SM[:, r, :], GVB[:, h * 128:(h + 1) * 128], d_ == 0, False, [bSM[r], bGVB], [bps[3]])
                if d_ == 0:
                    mm(oreg, QKTT[:, d_, 0, h, :], SBF[0:64, 4 + h, :], False, False, [bQKTT[d_], bSBF], [bps[3]])
                else:
                    mm(oreg, QKTT[:, d_, 0, h, :], SBGc[:, h, :], False, True, [bQKTT[d_], bSBc], [bps[3]])
        state_update(0, 0, 1)
        for half in range(2):
            pv = psf[2 + half][:, :]
            act(JUNK[:, half * 512:(half + 1) * 512], pv, AF.Square, [bps[2 + half]], [bJUNK])
        red("dve", ST[:, 0:8], JUNK[:].rearrange("p (h e) -> p h e", h=8), ALU.add, [bJUNK], [bST])
        ts("dve", ST[:, 0:8], ST[:, 0:8], 1.0 / 128, ALU.mult, [bST], [bST], EPS, ALU.add)
        act(ST[:, 0:8], ST[:, 0:8], AF.Sqrt, [bST], [bST])
        P.op("dve", lambda: nc.vector.reciprocal(out=ST[:, 0:8], in_=ST[:, 0:8]), [bST], [bST])
        for half in range(2):
            tt("dve", JUNK[:, half * 512:(half + 1) * 512].rearrange("p (h e) -> p h e", h=4),
               psf[2 + half][:, :].rearrange("p (h e) -> p h e", h=4),
               ST[:, half * 4:(half + 1) * 4].unsqueeze(2).to_broadcast([128, 4, 128]), ALU.mult,
               [bps[2 + half], bST], [bJUNK])
        tt("dve", YB[:], JUNK[:], SGATE[:], ALU.mult, [bJUNK, bSGATE], [bYB])
        for k in range(8):
            tr(psb[:, k * 128:(k + 1) * 128], YB[:, k * 128:(k + 1) * 128], IDB[:], [bYB, bIDB], [bpsb])
        cp("act", YT[:].rearrange("p a b -> p (a b)"), psb[:, :], [bpsb], [bYT])
        for g in range(2):
            ws = wload(w_out_s[:, g * 512:(g + 1) * 512], 512)
            for k in range(8):
                mm(psf[g][:, :], YT[:, k, :], WB[:, ws, k, :], k == 0, k == 7, [bYT, bWB[ws]], [bps[g]])
            tt("dve", X1D[:, pc, g * 512:(g + 1) * 512], psf[g][:, :], XT[:, s, g * 512:(g + 1) * 512], ALU.add,
               [bps[g], bXT[s]], [bX1[pc]])
        if stage == "x1":
            finals.append(dma("sp", out_d[c * 128:(c + 1) * 128, :], X1D[:, pc, :], [bX1[pc]], (), "out"))
        P.scope_fence(bl)
        mx.close()
        if stage == "x1":
            return
        pr = ExitStack()
        bl = []
        H2 = sb("H2", [128, D], F32, pr)
        bH2 = nb()
        IDXF_ = None
        bIDXF = nb()
        GWV = GWD[:, pc, :].rearrange("p (a b) -> p a b", a=8)
        YT = sb("H2T", [128, 8, 128], BF16, pr)
        bYT = nb()
        QT = sb("QT", [128, 16, 128], BF16, pr)
        bQT = nb()
        S12U = sb("S12U", [128, 2048], F32, pr)
        bS12 = nb()
        bSg = [nb() for _ in range(16)]
        bVg = [nb() for _ in range(16)]
        bIg = [nb() for _ in range(16)]
        bTh = [nb() for _ in range(8)]
        bPh = [nb() for _ in range(8)]
        bCAND = bS12
        bOH = bS12
        S12 = S12U[:].rearrange("p (a b) -> p a b", a=16)
        CAND = S12U[:].rearrange("p (a b) -> p a b", a=8)
        OH = S12U[:].rearrange("p (h a b) -> p h a b", h=8, a=16)
        SCR = sb("SCR", [128, 2, 256], F32, pr)
        bSCR = [nb(), nb()]
        V12 = sb("V12", [128, 16, 16], F32, pr)
        I12 = sb("I12", [128, 16, 16], U32, pr)
        I12F = sb("I12F", [128, 16, 16], F32, pr)
        bV12 = nb()
        TSV = sb("TSV", [128, 8, 16], F32, pr)
        POS = sb("POS", [128, 8, 16], U32, pr)
        PAF = sb("PAF", [128, 3, 8, 16], F32, pr)
        bTS = nb()
        ISEL = sb("ISEL", [128, 2, 8, 16], F32, pr)
        IDXF = sb("IDXF", [128, 128], F32, pr)
        act(JUNK[:], X1D[:, pc, :], AF.Square, [bX1[pc]], [bJUNK, bST], accum=ST[:, 0:1])
        ts("dve", ST[:, 1:2], ST[:, 0:1], 1.0 / D, ALU.mult, [bST], [bST], EPS, ALU.add)
        act(ST[:, 2:3], ST[:, 1:2], AF.Sqrt, [bST], [bST])
        P.op("dve", lambda: nc.vector.reciprocal(out=ST[:, 3:4], in_=ST[:, 2:3]), [bST], [bST])
        stt("dve", H2[:], X1D[:, pc, :], ST[:, 3:4], A2, ALU.mult, ALU.mult, [bX1[pc], bST, bROWS], [bH2])
        tt("dve", H2[:], H2[:], B2, ALU.add, [bH2, bROWS], [bH2])
        cp("act", H2BD[:, pc, :], H2[:], [bH2], [bH2B[pc]])
        for k in range(8):
            tr(psb[:, k * 128:(k + 1) * 128], H2BD[:, pc, k * 128:(k + 1) * 128], IDB[:], [bH2B[pc], bIDB], [bpsb])
        cp("act", YT[:].rearrange("p a b -> p (a b)"), psb[:, :], [bpsb], [bYT])
        for g4 in range(4):
            ws = wload(w_q_s[:, g4 * 512:(g4 + 1) * 512], 512)
            pb = 2 + g4 % 2
            for gg in range(4):
                for k in range(8):
                    mm(psf[pb][:, gg * 128:(gg + 1) * 128], WB[:, ws, k, gg * 128:(gg + 1) * 128], YT[:, k, :],
                       k == 0, k == 7, [bYT, bWB[ws]], [bps[pb]])
            cp("act", QT[:, g4 * 4:(g4 + 1) * 4, :].rearrange("p a b -> p (a b)"),
               psf[pb][:, :], [bps[pb]], [bQT])
        for g4 in range(4):
            pb = g4 % 2
            for gg in range(4):
                g = g4 * 4 + gg
                mm(psf[pb][:, gg * 128:(gg + 1) * 128], QT[:, g, :], KT[:, g, :], True, True, [bQT, bKT], [bps[pb]])
            cp("act", S12U[:, g4 * 512:(g4 + 1) * 512],
               psf[pb][:, :], [bps[pb]], [bS12] + bSg[g4 * 4:(g4 + 1) * 4])
        for g in range(16):
            P.op("dve", (lambda g=g: nc.vector.max(out=V12[:, g, 0:8], in_=S12[:, g, :])), [bSg[g]], [bVg[g]])
        for g in range(16):
            P.op("dve", (lambda g=g: nc.vector.max_index(out=I12[:, g, 0:8], in_max=V12[:, g, 0:8], in_values=S12[:, g, :])),
                 [bSg[g], bVg[g]], [bIg[g]])
        for g in range(16):
            P.op("dve", (lambda g=g: nc.vector.match_replace(out=S12[:, g, :], in_to_replace=V12[:, g, 0:8],
                                                             in_values=S12[:, g, :], imm_value=NEG)),
                 [bVg[g]], [bSg[g]])
        for g in range(16):
            P.op("dve", (lambda g=g: nc.vector.max(out=V12[:, g, 8:16], in_=S12[:, g, :])), [bSg[g]], [bVg[g]])
        for g in range(16):
            P.op("dve", (lambda g=g: nc.vector.max_index(out=I12[:, g, 8:16], in_max=V12[:, g, 8:16],
                                                         in_values=S12[:, g, :])), [bSg[g], bVg[g]], [bIg[g]])
        cp("dve", I12F[:].rearrange("p a b -> p (a b)"), I12[:].rearrange("p a b -> p (a b)"), bIg, [bV12])
        V3 = V12[:].rearrange("p (h two) k -> p h two k", two=2)
        tt("dve", OH,
           V3[:, :, 0, :].unsqueeze(3).to_broadcast([128, 8, 16, 16]),
           V3[:, :, 1, :].unsqueeze(2).to_broadcast([128, 8, 16, 16]), ALU.add, bVg, bSg)
        bCh = [[bSg[2 * h], bSg[2 * h + 1]] for h in range(8)]
        for h in range(8):
            P.op("dve", (lambda h=h: nc.vector.max(out=TSV[:, h, 0:8], in_=CAND[:, h, :])), bCh[h], [bTh[h]])
        for h in range(8):
            P.op("dve", (lambda h=h: nc.vector.max_index(out=POS[:, h, 0:8], in_max=TSV[:, h, 0:8], in_values=CAND[:, h, :])),
                 bCh[h] + [bTh[h]], [bPh[h]])
        for h in range(8):
            P.op("dve", (lambda h=h: nc.vector.match_replace(out=CAND[:, h, :], in_to_replace=TSV[:, h, 0:8],
                                                             in_values=CAND[:, h, :], imm_value=NEG)),
                 [bTh[h]], bCh[h])
        for h in range(8):
            P.op("dve", (lambda h=h: nc.vector.max(out=TSV[:, h, 8:16], in_=CAND[:, h, :])), bCh[h], [bTh[h]])
        for h in range(8):
            P.op("dve", (lambda h=h: nc.vector.max_index(out=POS[:, h, 8:16], in_max=TSV[:, h, 8:16],
                                                         in_values=CAND[:, h, :])), bCh[h] + [bTh[h]], [bPh[h]])
        P.op("dve", (lambda: nc.vector.tensor_copy(out=PAF[:, 2].rearrange("p a b -> p (a b)"),
                                                   in_=POS[:].rearrange("p a b -> p (a b)"))),
             bPh + bTh + bSg, [bTS, bS12], cost=0.3)
        tt("dve", OH, PAF[:, 2].unsqueeze(3).to_broadcast([128, 8, 16, 16]),
           THR16.unsqueeze(1).unsqueeze(1).to_broadcast([128, 8, 16, 16]), ALU.is_ge, [bTS, bC], [bOH])
        red("dve", PAF[:, 0], OH, ALU.add, [bOH], [bTS])
        stt("dve", PAF[:, 1].rearrange("p a b -> p (a b)"), PAF[:, 0].rearrange("p a b -> p (a b)"), -16.0,
            PAF[:, 2].rearrange("p a b -> p (a b)"), ALU.mult, ALU.add, [bTS], [bTS])
        I3 = I12F[:].rearrange("p (h two) k -> p h two k", two=2)
        for w in range(2):
            tt("dve", OH, PAF[:, w].unsqueeze(3).to_broadcast([128, 8, 16, 16]),
               IOTA16.unsqueeze(1).unsqueeze(1).to_broadcast([128, 8, 16, 16]), ALU.is_equal, [bTS, bC], [bOH])
            tt("dve", OH, OH, I3[:, :, w, :].unsqueeze(2).to_broadcast([128, 8, 16, 16]), ALU.mult,
               [bOH, bV12], [bOH])
            red("dve", ISEL[:, w], OH, ALU.add, [bOH], [bTS])
        stt("dve", IDXF[:], ISEL[:, 0].rearrange("p a b -> p (a b)"), 128.0, ISEL[:, 1].rearrange("p a b -> p (a b)"),
            ALU.mult, ALU.add, [bTS], [bIDXF])
        cp("dve", IDXD[:, pc, :], IDXF[:], [bIDXF], [bIDX[pc]])
        tt("dve", GWV, TSV[:], TSV[:, :, 0:1].to_broadcast([128, 8, 16]), ALU.subtract, [bTS], [bGW[pc]])
        act(GWD[:, pc, :], GWD[:, pc, :], AF.Exp, [bGW[pc]], [bGW[pc]])
        red("dve", ST[:, 0:8], GWV, ALU.add, [bGW[pc]], [bST])
        P.op("dve", lambda: nc.vector.reciprocal(out=ST[:, 0:8], in_=ST[:, 0:8]), [bST], [bST])
        tt("dve", GWV, GWV, ST[:, 0:8].unsqueeze(2).to_broadcast([128, 8, 16]), ALU.mult, [bGW[pc], bST], [bGW[pc]])
        P.scope_fence(bl)
        pr.close()

    def gather(c):
        pc = c % 2
        LA = []
        LB = []
        BLK = 4
        nblk = 128 // BLK
        assert NG >= 2 * BLK + 1
        P.insts = LA
        memset("pool", ACTV[:], 0.0, bA)
        GW2 = GWD[:, pc, :]
        for b_ in range(nblk):
            P.insts = LA
            for k in range(BLK):
                j = b_ * BLK + k
                r = j % NG
                P.op("pool", (lambda j=j, r=r: nc.gpsimd.indirect_dma_start(
                    out=UV[:, r, :], out_offset=None, in_=du_s,
                    in_offset=bass.IndirectOffsetOnAxis(ap=IDXD[:, pc, j:j + 1], axis=0))),
                    [bIDX[pc], bTAB], [bUV[r]], dma=True, slot="uv%d" % r, cost=1.25, lat=5.0)
                stt("dve", JB[:], UV[:, r, 0:D], 1.0, H2BD[:, pc, :], ALU.mult, ALU.mult,
                    [bUV[r], bH2B[pc]], [bJB, bA[j]], accum=ACTV[:, j:j + 1])
            P.insts = LB
            js = slice(b_ * BLK, (b_ + 1) * BLK)
            act(COEF[:, js], ACTV[:, js], AF.Gelu, [bA[b_ * BLK + k] for k in range(BLK)], [bCf[b_]])
            tt("dve", COEF[:, js], COEF[:, js], GW2[:, js], ALU.mult, [bCf[b_], bGW[pc]], [bCf[b_]])
            for k in range(BLK):
                j = b_ * BLK + k
                r = j % NG
                dsl = j % 4
                act(DGS[:, dsl, :], IDB[:], AF.Copy, [bIDB, bCf[b_]], [bDGS[dsl]], scale=COEF[:, j:j + 1])
                for g in range(2):
                    mm(psf[5 + g][:, :], DGS[:, dsl, :], UV[:, r, D + g * 512:D + (g + 1) * 512],
                       j == 0, j == 127, [bDGS[dsl], bUV[r]], [bps[5 + g]])
        P.insts = LB
        for g in range(2):
            tt("dve", ACC[:, g * 512:(g + 1) * 512], psf[5 + g][:, :], G2[:, g * 512:(g + 1) * 512], ALU.mult,
               [bps[5 + g], bROWS], [bACC])
        tt("dve", ACC[:], ACC[:], X1D[:, pc, :], ALU.add, [bACC, bX1[pc]], [bACC])
        act(JB[:], ACC[:], AF.Square, [bACC], [bJB, bST2], accum=ST2[:, 0:1])
        ts("dve", ST2[:, 1:2], ST2[:, 0:1], 1.0 / D, ALU.mult, [bST2], [bST2], EPS, ALU.add)
        act(ST2[:, 2:3], ST2[:, 1:2], AF.Sqrt, [bST2], [bST2])
        P.op("dve", lambda: nc.vector.reciprocal(out=ST2[:, 3:4], in_=ST2[:, 2:3]), [bST2], [bST2])
        stt("dve", ACC[:], ACC[:], ST2[:, 3:4], NF, ALU.mult, ALU.mult, [bACC, bST2, bROWS], [bACC])
        finals.append(dma("sp", out_d[c * 128:(c + 1) * 128, :], ACC[:], [bACC], (), "out"))
        for L_ in (LA, LB):
            P._chains.append(L_)
            P._bias.append(0.0)
        P.insts = []

    P.begin_chains()
    P.next_chain(MIXBIAS)
    mix(0)
    print("sbuf in pass3", nc.sbuf_bytes_remaining)
    if stage != "x1":
        for c in range(n_own):
            P.next_chain(0.0)
            gather(c)
            P.next_chain(MIXBIAS)
            if c + 1 < n_own:
                mix(c + 1)
    else:
        for c in range(1, n_own):
            P.next_chain()
            mix(c)
    P.end_chains(6)

    P.barrier()
    p3.close()
    es.close()
    return nc, P.stats


def _consts():
    p = np.arange(128, dtype=np.float32)
    J = p[:, None]
    I = p[None, :]
    cmats = np.stack([
        np.eye(128, dtype=np.float32),
        (J <= I).astype(np.float32),
        (J >= I).astype(np.float32),
        np.maximum(I - J, 0.0),
        np.maximum(J - I, 0.0),
        (J > I).astype(np.float32),
        (J < I).astype(np.float32),
        np.broadcast_to(I + 1.0, (128, 128)),
    ], axis=1).astype(np.float32)
    csm = np.zeros((128, 48), np.float32)
    csm[:, 0:4] = (127.0 - p)[:, None]
    csm[:, 4:8] = p[:, None]
    csm[:, 8:24] = np.arange(16, dtype=np.float32)[None, :]
    csm[:, 24] = 1.0
    csm[:, 32:48] = 16.0 * (np.arange(16, dtype=np.float32) + 1.0)[None, :]
    r, c = np.meshgrid(np.arange(128, dtype=np.float32), np.arange(64, dtype=np.float32), indexing="ij")
    inv = (10000.0 ** (-np.arange(32, dtype=np.float32) / 32)).astype(np.float32)
    ang = np.concatenate([r.reshape(-1, 1) * inv, c.reshape(-1, 1) * inv], axis=-1).astype(np.float32)
    rope = np.concatenate([np.cos(ang), np.sin(ang)], axis=-1).astype(np.float32)
    return cmats.reshape(128, 1024), csm, rope


def make_in_maps(inputs):
    f = lambda a: np.ascontiguousarray(np.asarray(a, dtype=np.float32))
    x = f(inputs["x"]); c = f(inputs["c"]); ctx = f(inputs["ctx"]); c_ctx = f(inputs["c_ctx"])
    cm, csm, rope = _consts()
    w_mod = f(inputs["w_mod"][0]); b_mod = f(inputs["b_mod"][0])
    bmodT = f(b_mod.reshape(48, 128).T)
    n1T = f(np.asarray(inputs["norm1_w"][0]).reshape(8, 128).T)
    n2row = f(np.asarray(inputs["norm2_w"][0]).reshape(1, D))
    nfrow = f(np.asarray(inputs["norm_f_w"]).reshape(1, D))
    w_in = f(inputs["w_in"][0])
    retd = f(np.concatenate([np.asarray(inputs["ret_decay_f"][0]), np.asarray(inputs["ret_decay_b"][0])]).reshape(1, 8))
    gkup = np.zeros((32, 512), np.float32)
    gkup[0:16, 0:256] = np.asarray(inputs["gla_gk_up_f"][0])
    gkup[16:32, 256:512] = np.asarray(inputs["gla_gk_up_b"][0])
    gkbias = f(np.concatenate([np.asarray(inputs["gla_gk_bias_f"][0]), np.asarray(inputs["gla_gk_bias_b"][0])]).reshape(1, 512))
    w_out = f(inputs["w_out"][0]); w_q = f(inputs["peer_w_q"][0])
    k1 = np.asarray(inputs["peer_k1"][0]); k2 = np.asarray(inputs["peer_k2"][0])
    k12 = np.stack([k1, k2], axis=1).reshape(16, 128, 128)
    k12T = f(k12.transpose(2, 0, 1).reshape(128, 2048))
    down = f(inputs["peer_down"][0]); up = f(inputs["peer_up"][0])
    maps = []
    for core in range(8):
        b, seg = core // 4, core % 4
        cT = f(np.concatenate([c[b].reshape(8, 128).T, c_ctx.reshape(8, 128).T], axis=1))
        sel = np.zeros((128, 4), np.float32)
        sel[:, seg] = 1.0
        maps.append(dict(
            xb=x[b], xo=f(x[b, seg * 2048:(seg + 1) * 2048]), ctx=ctx[b], cT=cT, w_mod=w_mod, bmodT=bmodT,
            n1T=n1T, n2row=n2row, nfrow=nfrow, w_in=w_in, retd=retd, gkup=gkup, gkbias=gkbias,
            w_out=w_out, w_q=w_q, k12T=k12T, down=down, up=up, cm=cm, cs=csm,
            ropeb=rope, ropeo=f(rope[seg * 2048:(seg + 1) * 2048]), segsel=sel))
    return maps


def kernel(**inputs):
    nc, _ = build("full")
    maps = make_in_maps(inputs)
    res = run_bass_kernel_spmd(nc, maps, core_ids=list(range(8)))
    out = np.zeros((2, SEQ, D), np.float32)
    for core in range(8):
        b, seg = core // 4, core % 4
        out[b, seg * 2048:(seg + 1) * 2048] = res.results[core]["out"]
    return out
```

```python
import numpy as np
from contextlib import ExitStack
import concourse.bass as bass
import concourse.mybir as mybir
from concourse.bass_utils import run_bass_kernel_spmd

F32 = mybir.dt.float32
BF16 = mybir.dt.bfloat16
I32 = mybir.dt.int32
U32 = mybir.dt.uint32
ALU = mybir.AluOpType
AF = mybir.ActivationFunctionType
AX = mybir.AxisListType

D = 1024
SEQ = 8192
NT = 64
OT = 16
INC = 3616
EPS = 1e-6
MIXBIAS = 0.0
SYNC = 1.5
NEG = -1e30


class Buf:
    __slots__ = ("w", "r")
    registry = []

    def __init__(self):
        self.w = None
        self.r = []
        Buf.registry.append(self)


class Inst:
    __slots__ = ("eng", "fn", "deps", "dma", "need", "sem", "val", "slot", "seq", "cost", "lat")

    def __init__(self, eng, fn, deps, dma, slot):
        self.eng = eng
        self.fn = fn
        self.deps = deps
        self.dma = dma
        self.need = False
        self.sem = None
        self.val = None
        self.slot = slot


class Prog:
    def __init__(self, nc, es):
        self.nc = nc
        self.es = es
        self.insts = []
        self.engs = {"pe": nc.tensor, "dve": nc.vector, "act": nc.scalar,
                     "pool": nc.gpsimd, "sp": nc.sync}

    def op(self, eng, fn, reads=(), writes=(), dma=False, slot=None, cost=0.3, lat=0.0):
        deps = []
        for b in reads:
            if b.w is not None:
                deps.append(b.w)
        for b in writes:
            if b.w is not None:
                deps.append(b.w)
            deps.extend(b.r)
        seen = set()
        d2 = []
        for d in deps:
            if id(d) not in seen:
                seen.add(id(d))
                d2.append(d)
        ins = Inst(eng, fn, d2, dma, slot)
        self.seq = getattr(self, "seq", 0) + 1
        ins.seq = self.seq
        ins.cost = cost
        ins.lat = lat
        for d in d2:
            if dma or d.dma or not (d.eng == "pe" and eng == "pe"):
                d.need = True
        for b in reads:
            b.r.append(ins)
        for b in writes:
            b.w = ins
            b.r = []
        self.insts.append(ins)
        return ins

    def scope_fence(self, bufs):
        if not hasattr(self, "fence"):
            self.fence = {}
        for b in bufs:
            for ins in ([b.w] if b.w is not None else []) + list(b.r):
                key = ("d", ins.slot) if ins.dma else ins.eng
                cur = self.fence.get(key)
                if cur is None or cur.seq < ins.seq:
                    self.fence[key] = ins

    def newbuf(self):
        b = Buf()
        b.r = list(getattr(self, "fence", {}).values())
        return b

    @staticmethod
    def merge(A, B):
        inA = {id(a) for a in A}
        placed = set()
        out = []
        ia = ib = 0
        while ia < len(A) or ib < len(B):
            takeB = ib < len(B) and (ia >= len(A) or ib * len(A) <= ia * len(B))
            if takeB:
                b = B[ib]
                if all((id(d) not in inA) or (id(d) in placed) for d in b.deps):
                    out.append(b)
                    ib += 1
                    continue
            if ia < len(A):
                a = A[ia]
                out.append(a)
                placed.add(id(a))
                ia += 1
            else:
                out.append(B[ib])
                ib += 1
        return out

    @staticmethod
    def schedule(A, B, sync=1.0):
        from collections import deque
        member = {}
        for x in A:
            member[id(x)] = 0
        for x in B:
            member[id(x)] = 1
        q = [dict(), dict()]
        for ci, L in enumerate((A, B)):
            for x in L:
                q[ci].setdefault(x.eng, deque()).append(x)
        done = {}
        efree = {}
        out = []
        n = len(A) + len(B)
        while len(out) < n:
            best = None
            for ci in range(2):
                for e, dq in q[ci].items():
                    if not dq:
                        continue
                    x = dq[0]
                    t = efree.get(e, 0.0)
                    ok = True
                    for d in x.deps:
                        if id(d) in member:
                            dt_ = done.get(id(d))
                            if dt_ is None:
                                ok = False
                                break
                            lt = dt_ + (0.0 if (d.eng == e and e == "pe" and not d.dma) else sync)
                            if lt > t:
                                t = lt
                    if ok:
                        key = t - (bias[ci] if bias else 0.0)
                        if best is None or key < best[0]:
                            best = (key, ci, e, x, t)
            assert best is not None, "scheduler deadlock"
            _, ci, e, x, t = best
            q[ci][e].popleft()
            efree[e] = t + x.cost
            done[id(x)] = t + x.cost + x.lat
            out.append(x)
        return out

    @staticmethod
    def schedule_chains(chains, window=3, sync=1.0, bias=None):
        from collections import deque
        member = set()
        qs = []
        for L in chains:
            qd = {}
            for x in L:
                member.add(id(x))
                qd.setdefault(x.eng, deque()).append(x)
            qs.append(qd)
        remaining = [len(L) for L in chains]
        done = {}
        efree = {}
        out = []
        n = sum(remaining)
        lo = 0
        while len(out) < n:
            while lo < len(chains) and remaining[lo] == 0:
                lo += 1
            best = None
            for ci in range(lo, min(lo + window, len(chains))):
                for e, dq in qs[ci].items():
                    if not dq:
                        continue
                    x = dq[0]
                    t = efree.get(e, 0.0)
                    ok = True
                    for d in x.deps:
                        if id(d) in member:
                            dt_ = done.get(id(d))
                            if dt_ is None:
                                ok = False
                                break
                            lt = dt_ + (0.0 if (d.eng == e and e == "pe" and not d.dma) else sync)
                            if lt > t:
                                t = lt
                    if ok:
                        key = t - (bias[ci] if bias else 0.0)
                        if best is None or key < best[0]:
                            best = (key, ci, e, x, t)
            assert best is not None, "scheduler deadlock"
            _, ci, e, x, t = best
            qs[ci][e].popleft()
            remaining[ci] -= 1
            efree[e] = t + x.cost
            done[id(x)] = t + x.cost + x.lat
            out.append(x)
        return out

    def begin_chains(self):
        self._base = self.insts
        self._chains = []
        self._bias = []
        self._curbias = 0.0
        self.insts = []

    def next_chain(self, bias=0.0):
        if self.insts:
            self._chains.append(self.insts)
            self._bias.append(self._curbias)
        self._curbias = bias
        self.insts = []

    def end_chains(self, window=3):
        self.next_chain()
        self.insts = self._base + Prog.schedule_chains(self._chains, window, sync=SYNC, bias=self._bias)
        self._chains = []

    def flush(self):
        nc = self.nc
        if not hasattr(self, "esem"):
            self.esem = {}
            self.ecnt = {}
            for e in self.engs:
                self.esem[e] = self.es.enter_context(nc.semaphore("s_" + e))
                self.ecnt[e] = 0
            self.slot_sem = {}
            self.slot_cnt = {}
            self.known = {e: {} for e in self.engs}
            self.total = 0
        esem, ecnt, slot_sem, slot_cnt, known = self.esem, self.ecnt, self.slot_sem, self.slot_cnt, self.known
        for b in Buf.registry:
            if b.w is not None:
                b.w.need = True
            for r_ in b.r:
                r_.need = True
        last = {}
        for ins in self.insts:
            if ins.dma:
                ins.need = True
            last[ins.eng] = ins
        for ins in last.values():
            ins.need = True
        self.total += len(self.insts)
        for ins in self.insts:
            eng = self.engs[ins.eng]
            kn = known[ins.eng]
            for d in ins.deps:
                if d.sem is None or (d.eng == "pe" and ins.eng == "pe" and not d.dma):
                    continue
                key = id(d.sem)
                if kn.get(key, 0) < d.val:
                    eng.wait_ge(d.sem, d.val)
                    kn[key] = d.val
            bi = ins.fn()
            if ins.need:
                if ins.dma:
                    s = ins.slot
                    if s not in slot_sem:
                        slot_sem[s] = self.es.enter_context(nc.semaphore("d_%s" % (s,)))
                        slot_cnt[s] = 0
                    slot_cnt[s] += 16
                    bi.then_inc(slot_sem[s], 16)
                    ins.sem = slot_sem[s]
                    ins.val = slot_cnt[s]
                else:
                    ecnt[ins.eng] += 1
                    bi.then_inc(esem[ins.eng], 1)
                    ins.sem = esem[ins.eng]
                    ins.val = ecnt[ins.eng]
        self.insts = []
        self.stats = dict(n=self.total, cnt=dict(ecnt), nslots=len(slot_sem))

    def barrier(self):
        self.flush()
        for e, eng in self.engs.items():
            kn = self.known[e]
            for e2 in self.engs:
                if e2 != e and self.ecnt[e2] > 0 and kn.get(id(self.esem[e2]), 0) < self.ecnt[e2]:
                    eng.wait_ge(self.esem[e2], self.ecnt[e2])
                    kn[id(self.esem[e2])] = self.ecnt[e2]
            for s_, sem in self.slot_sem.items():
                if kn.get(id(sem), 0) < self.slot_cnt[s_]:
                    eng.wait_ge(sem, self.slot_cnt[s_])
                    kn[id(sem)] = self.slot_cnt[s_]


def build(stage="full", n_own=OT, n_state=True):
    nc = bass.Bass("TRN2", target_bir_lowering=False)

    def din(name, shape, dt=F32):
        return nc.dram_tensor(name, list(shape), dt, kind="ExternalInput").ap()

    xb = din("xb", [SEQ, D])
    xo = din("xo", [OT * 128, D])
    ctx = din("ctx", [256, D])
    cT = din("cT", [128, 16])
    w_mod = din("w_mod", [D, 6 * D])
    bmodT = din("bmodT", [128, 48])
    n1T = din("n1T", [128, 8])
    n2row = din("n2row", [1, D])
    nfrow = din("nfrow", [1, D])
    w_in = din("w_in", [D, INC])
    retd = din("retd", [1, 8])
    gkup = din("gkup", [32, 512])
    gkbias = din("gkbias", [1, 512])
    w_out = din("w_out", [D, D])
    w_q = din("w_q", [D, 2048])
    k12T = din("k12T", [128, 2048])
    down = din("down", [16384, D])
    up = din("up", [16384, D])
    cm = din("cm", [128, 8 * 128])
    cs = din("cs", [128, 48])
    ropeb = din("ropeb", [SEQ, 128])
    ropeo = din("ropeo", [OT * 128, 128])
    segsel = din("segsel", [128, 4])
    out_d = nc.dram_tensor("out", [OT * 128, D], F32, kind="ExternalOutput").ap()
    w_in_s = nc.dram_tensor("w_in_s", [D, 4096], BF16, kind="Internal").ap()
    w_q_s = nc.dram_tensor("w_q_s", [D, 2048], BF16, kind="Internal").ap()
    w_out_s = nc.dram_tensor("w_out_s", [D, D], BF16, kind="Internal").ap()
    sbr_s = nc.dram_tensor("sbr_s", [OT, 128, 512], BF16, kind="Internal").ap()
    sbg_s = nc.dram_tensor("sbg_s", [OT, 64, 512], BF16, kind="Internal").ap()
    du_s = nc.dram_tensor("du_s", [16384, 2 * D], BF16, kind="Internal").ap()

    es = ExitStack()
    Buf.registry = []
    P = Prog(nc, es)
    E = P.engs
    p0 = ExitStack()

    PERSIST = [
        ("CM", [128, 8, 128], F32), ("CS", [128, 48], F32), ("IDB", [128, 128], BF16), ("ONESR", [1, 128], F32),
        ("IOTAB", [128, 128], F32), ("LG", [128, 8], F32), ("MASK", [128, 8, 128], F32), ("QDC", [128, 8, 128], F32),
        ("KD", [128, 8], F32), ("CD", [128, 8], F32), ("GKU", [32, 512], BF16), ("GKB", [1, 512], F32),
        ("MODT", [128, 48, 2], F32), ("AB1", [128, 4, 8], F32), ("ROWS", [128, 4, D], F32),
        ("WB", [128, 3, 8, 512], BF16), ("KT", [128, 16, 128], BF16),
        ("SR", [128, 2, 4, 128], F32), ("SG", [64, 2, 4, 128], F32), ("SBR", [128, 2, 4, 128], BF16), ("SBG", [64, 2, 4, 128], BF16),
        ("SEL", [128, 4], F32), ("XT", [128, 2, D], F32), ("ROPE", [128, 2, 128], F32), ("JUNK", [128, D], F32),
        ("ST", [128, 8], F32), ("XH", [128, 1, D], F32), ("HT", [128, 2, 8, 128], BF16), ("KTMP", [128, 4, 128], F32),
        ("KDB", [128, 4, 128], BF16), ("RVB", [128, 512], BF16),
        ("GVB", [128, 512], BF16), ("LRT", [32, 128], BF16), ("LL", [128, 512], F32), ("EG", [128, 256], F32),
        ("KDG", [128, 256], BF16), ("DG", [64, 4], F32),
    ]
    pre = {}
    uniq = [0]
    for (nm, shp, dt_) in PERSIST:
        pre[nm] = es.enter_context(nc.sbuf_tensor(nm, list(shp), dt_))

    def sb(name, shape, dt=F32, scope=None):
        if scope is None:
            t = pre[name]
            assert list(t.shape) == list(shape) and t.dtype == dt, name
            return t
        uniq[0] += 1
        return scope.enter_context(nc.sbuf_tensor("%s_%d" % (name, uniq[0]), list(shape), dt))

    def fsz(ap):
        n_ = 1
        for d_ in ap.shape[1:]:
            n_ *= int(d_)
        return n_

    def dma(q, out, in_, R, W, slot):
        nb_ = fsz(out) * 128 * 4
        return P.op(q, lambda: E[q].dma_start(out=out, in_=in_), R, W, dma=True, slot=slot,
                    cost=(1.0 if q == "pool" else 0.15), lat=2.5 + nb_ / 2.0e5)

    def ecost(eng, out, psum=False):
        n_ = fsz(out)
        if eng == "dve":
            return 0.1 + n_ / (1650.0 if out.dtype == BF16 else 960.0)
        if eng == "act":
            return 0.25 + n_ / 1200.0
        return 0.15 + n_ / 500.0

    def tt(eng, out, a, b, op, R, W):
        return P.op(eng, lambda: E[eng].tensor_tensor(out=out, in0=a, in1=b, op=op), R, W, cost=ecost(eng, out))

    def ts(eng, out, a, s1, op0, R, W, s2=None, op1=None):
        if op1 is None:
            return P.op(eng, lambda: E[eng].tensor_scalar(out=out, in0=a, scalar1=s1, scalar2=None, op0=op0), R, W,
                        cost=ecost(eng, out))
        return P.op(eng, lambda: E[eng].tensor_scalar(out=out, in0=a, scalar1=s1, scalar2=s2, op0=op0, op1=op1), R, W,
                    cost=ecost(eng, out))

    def stt(eng, out, a, s, b, op0, op1, R, W, accum=None):
        if accum is None:
            return P.op(eng, lambda: E[eng].scalar_tensor_tensor(out=out, in0=a, scalar=s, in1=b, op0=op0, op1=op1), R, W,
                        cost=ecost(eng, out))
        return P.op(eng, lambda: E[eng].scalar_tensor_tensor(out=out, in0=a, scalar=s, in1=b, op0=op0, op1=op1, accum_out=accum), R, W,
                    cost=ecost(eng, out))

    def act(out, in_, func, R, W, bias=None, scale=None, accum=None):
        kw = {}
        if bias is not None:
            kw["bias"] = bias
        if scale is not None:
            kw["scale"] = scale
        if accum is not None:
            kw["accum_out"] = accum
        return P.op("act", lambda: nc.scalar.activation(out=out, in_=in_, func=func, **kw), R, W, cost=ecost("act", out))

    def cp(eng, out, in_, R, W):
        if eng == "act":
            return P.op("act", lambda: nc.scalar.copy(out=out, in_=in_), R, W, cost=ecost("act", out))
        return P.op(eng, lambda: E[eng].tensor_copy(out=out, in_=in_), R, W, cost=ecost(eng, out))

    def mm(out, lhsT, rhs, start, stop, R, W):
        c_ = 0.02 + fsz(rhs) / 1300.0 * (4.0 if rhs.dtype == F32 else 1.0)
        return P.op("pe", lambda: nc.tensor.matmul(out, lhsT=lhsT, rhs=rhs, start=start, stop=stop), R, W, cost=c_)

    def tr(out, in_, ident, R, W):
        c_ = 0.02 + 128 / 1300.0 * (4.0 if in_.dtype == F32 else 1.0)
        return P.op("pe", lambda: nc.tensor.transpose(out=out, in_=in_, identity=ident), R, W, cost=c_)

    def memset(eng, ap, v, W):
        return P.op(eng, lambda: E[eng].memset(ap, v), (), W)

    def red(eng, out, in_, op, R, W):
        return P.op(eng, lambda: E[eng].tensor_reduce(out=out, in_=in_, axis=AX.X, op=op), R, W, cost=ecost(eng, in_))

    psf = [es.enter_context(nc.psum_tensor("ps%d" % i, [128, 512], F32)) for i in range(7)]
    psb = es.enter_context(nc.psum_tensor("psb", [128, 1024], BF16))
    bps = [Buf() for _ in range(7)]
    bpsb = Buf()

    CM = sb("CM", [128, 8, 128])
    CS = sb("CS", [128, 48])
    bC = Buf()
    dma("sp", CM[:].rearrange("p a b -> p (a b)"), cm, (), [bC], "c0")
    dma("sp", CS[:], cs, (), [bC], "c0")
    IDENT = CM[:, 0, :]
    SLE = CM[:, 1, :]
    SGE = CM[:, 2, :]
    RELF = CM[:, 3, :]
    RELB = CM[:, 4, :]
    SGT = CM[:, 5, :]
    SLT = CM[:, 6, :]
    IOTAF = CM[:, 7, :]
    COLF = CS[:, 0:4]
    COLB = CS[:, 4:8]
    IOTA16 = CS[:, 8:24]
    ONESC = CS[:, 24:25]
    THR16 = CS[:, 32:48]
    IDB = sb("IDB", [128, 128], BF16)
    bIDB = Buf()
    cp("dve", IDB[:], IDENT, [bC], [bIDB])
    ONESR = sb("ONESR", [1, 128])
    bONESR = Buf()
    memset("dve", ONESR[:], 1.0, [bONESR])
    IOTAB = sb("IOTAB", [128, 128])
    ts("dve", IOTAB[:], IOTAF, -1.0, ALU.mult, [bC], [bC], 129.0, ALU.add)

    RD = sb("RD", [128, 8], scope=p0)
    LG = sb("LG", [128, 8])
    bLG = Buf()
    dma("act", RD[:], retd.partition_broadcast(128), (), [bLG], "c1")
    act(LG[:], RD[:], AF.Exp, [bLG], [bLG], scale=-1.0)
    act(RD[:], LG[:], AF.Ln, [bLG], [bLG], bias=1.0)
    ts("dve", LG[:], RD[:], -1.0, ALU.mult, [bLG], [bLG])
    MASK = sb("MASK", [128, 8, 128])
    QDC = sb("QDC", [128, 8, 128])
    KD = sb("KD", [128, 8])
    CD = sb("CD", [128, 8])
    bDC = Buf()
    RSC = 128.0 ** -0.5
    for d_ in range(2):
        rel = RELF if d_ == 0 else RELB
        tri = SLE if d_ == 0 else SGE
        iot = IOTAF if d_ == 0 else IOTAB[:]
        for h in range(4):
            c = d_ * 4 + h
            act(MASK[:, c, :], rel, AF.Exp, [bC, bLG], [bDC], scale=LG[:, c:c + 1])
            stt("dve", MASK[:, c, :], MASK[:, c, :], RSC, tri, ALU.mult, ALU.mult, [bDC, bC], [bDC])
            act(QDC[:, c, :], iot, AF.Exp, [bC, bLG], [bDC], scale=LG[:, c:c + 1])
    tt("dve", KD[:, 0:4], COLF, LG[:, 0:4], ALU.mult, [bC, bLG], [bDC])
    tt("dve", KD[:, 4:8], COLB, LG[:, 4:8], ALU.mult, [bC, bLG], [bDC])
    act(KD[:], KD[:], AF.Exp, [bDC], [bDC])
    ts("dve", KD[:], KD[:], RSC, ALU.mult, [bDC], [bDC])
    act(CD[:], LG[:], AF.Exp, [bLG], [bDC], scale=128.0)

    STG = sb("STG", [128, 8, 512], scope=p0)
    bSTG = Buf()
    STGB = sb("STGB", [128, 8, 512], F32, scope=p0)
    bSTGB = Buf()
    GKU = sb("GKU", [32, 512], BF16)
    GKB = sb("GKB", [1, 512])
    bGK = Buf()
    dma("act", STG[0:32, 0, :], gkup, (), [bSTG], "stg")
    cp("dve", GKU[:], STG[0:32, 0, :], [bSTG], [bGK])
    dma("act", GKB[:], gkbias, (), [bGK], "c2")

    SC = sb("SC", [128, 16], scope=p0)
    bSC = Buf()
    dma("sp", SC[:], cT, (), [bSC], "c3")
    act(SC[:], SC[:], AF.Silu, [bSC], [bSC])
    MODT = sb("MODT", [128, 48, 2])
    bMOD = Buf()
    BM = sb("BM", [128, 48], scope=p0)
    dma("sp", BM[:], bmodT, (), [bMOD], "c4")
    SC3 = SC[:].rearrange("p (a k) -> p a k", a=2)
    for g in range(12):
        SGt, bSGt = (STG, bSTG) if g % 2 == 0 else (STGB, bSTGB)
        dma("sp" if g % 2 == 0 else "act", SGt[:], w_mod[:, g * 512:(g + 1) * 512].rearrange("(k p) n -> p k n", p=128),
            (), [bSGt], "stg%d" % (g % 2))
        pb = g % 2
        for c4 in range(4):
            for kk in range(8):
                mm(psf[pb][:, c4 * 2:c4 * 2 + 2], SGt[:, kk, c4 * 128:(c4 + 1) * 128], SC3[:, :, kk],
                   kk == 0, kk == 7, [bSGt, bSC], [bps[pb]])
        cp("dve", MODT[:, g * 4:(g + 1) * 4, :].rearrange("p a b -> p (a b)"), psf[pb][:, 0:8], [bps[pb]], [bMOD])
    tt("dve", MODT[:], MODT[:], BM[:].unsqueeze(2).to_broadcast([128, 48, 2]), ALU.add, [bMOD], [bMOD])
    N1 = sb("N1", [128, 8], scope=p0)
    dma("sp", N1[:], n1T, (), [bMOD], "c5")
    AB1 = sb("AB1", [128, 4, 8])
    bAB = Buf()
    for v in range(2):
        stt("dve", AB1[:, 2 * v, :], MODT[:, 8:16, v], 1.0, N1[:], ALU.add, ALU.mult, [bMOD], [bAB])
        cp("dve", AB1[:, 2 * v + 1, :], MODT[:, 0:8, v], [bMOD], [bAB])
    ROWS = sb("ROWS", [128, 4, D])
    bROWS = Buf()
    G1R = sb("G1R", [128, D], scope=p0)
    bG1R = Buf()
    CREP = sb("CREP", [128, 2, 128], scope=p0)
    bCREP = [Buf(), Buf()]
    N2R = sb("N2R", [128, D], scope=p0)
    dma("act", N2R[:], n2row.partition_broadcast(128), (), [bROWS], "c6")
    dma("act", ROWS[:, 3, :], nfrow.partition_broadcast(128), (), [bROWS], "c6")
    cnt = 0
    for (j, dst) in ((2, G1R[:]), (3, ROWS[:, 1, :]), (4, ROWS[:, 0, :]), (5, ROWS[:, 2, :])):
        for k in range(8):
            s = cnt % 2
            cnt += 1
            cp("dve", CREP[:, s, :], MODT[:, j * 8 + k, 0:1].to_broadcast([128, 128]), [bMOD], [bCREP[s]])
            mm(psf[2 + s][:, 0:128], CREP[:, s, :], IDENT, True, True, [bCREP[s], bC], [bps[2 + s]])
            cp("act", dst[:, k * 128:(k + 1) * 128], psf[2 + s][:, 0:128], [bps[2 + s]], [bG1R if j == 2 else bROWS])
    stt("dve", ROWS[:, 0, :], ROWS[:, 0, :], 1.0, N2R[:], ALU.add, ALU.mult, [bROWS], [bROWS])
    A2 = ROWS[:, 0, :]
    B2 = ROWS[:, 1, :]
    G2 = ROWS[:, 2, :]
    NF = ROWS[:, 3, :]

    WB = sb("WB", [128, 3, 8, 512], BF16)
    bWB = [Buf(), Buf(), Buf()]
    bWSall = []
    nconv = 0

    def conv(src, dst, ncols, fold=None):
        nonlocal nconv
        s = nconv % 2
        nconv += 1
        SGt, bSGt = (STG, bSTG) if s == 0 else (STGB, bSTGB)
        dma("sp", SGt[:, :, 0:ncols], src.rearrange("(k p) n -> p k n", p=128), (), [bSGt], "stg%d" % s)
        for kk in range(8):
            eng = "dve" if kk % 2 == 0 else "pool"
            if fold is None:
                cp(eng, WB[:, s, kk, 0:ncols], SGt[:, kk, 0:ncols], [bSGt], [bWB[s]])
            else:
                tt(eng, WB[:, s, kk, 0:ncols], SGt[:, kk, 0:ncols], fold, ALU.mult, [bSGt, bG1R], [bWB[s]])
        bw_ = Buf()
        bWSall.append(bw_)
        dma("act", dst.rearrange("(k p) n -> p k n", p=128), WB[:, s, :, 0:ncols], [bWB[s]], [bw_], "ws%d" % s)

    for g in range(8):
        n = 512 if g < 7 else 32
        conv(w_in[:, g * 512:g * 512 + n], w_in_s[:, g * 512:g * 512 + n], n)
    for g in range(4):
        conv(w_q[:, g * 512:(g + 1) * 512], w_q_s[:, g * 512:(g + 1) * 512], 512)
    for g in range(2):
        conv(w_out[:, g * 512:(g + 1) * 512], w_out_s[:, g * 512:(g + 1) * 512], 512, fold=G1R[:, g * 512:(g + 1) * 512])

    KT = sb("KT", [128, 16, 128], BF16)
    bKT = Buf()
    dma("sp", STG[:, 0:4, :].rearrange("p a b -> p (a b)"), k12T, (), [bSTG], "stg")
    cp("dve", KT[:].rearrange("p a b -> p (a b)"), STG[:, 0:4, :].rearrange("p a b -> p (a b)"), [bSTG], [bKT])
    P.barrier()
    print("sbuf after phase0", nc.sbuf_bytes_remaining)
    p0.close()
    p12 = ExitStack()
    WKV = sb("WKV", [128, 8, 1824], BF16, scope=p12)
    bWKV = Buf()
    for (o, c0, n) in ((0, 512, 512), (512, 1024, 512), (1024, 2304, 256), (1280, 2560, 512), (1792, 3584, 32)):
        dma("sp", WKV[:, :, o:o + n], w_in_s[:, c0:c0 + n].rearrange("(k p) n -> p k n", p=128), bWSall, [bWKV], "wkv")

    TF = sb("TF", [128, 2, 2048], F32, scope=p12)
    TBF = sb("TBF", [128, 2, 2048], BF16, scope=p12)
    bTF = [Buf(), Buf()]
    bTBF = [Buf(), Buf()]
    bTAB = Buf()
    tconv = [0]

    def table_chunk():
        i = tconv[0]
        if i >= 128:
            return
        tconv[0] += 1
        src = down if i < 64 else up
        c0 = 0 if i < 64 else D
        c = i % 64
        s_ = i % 2
        sv = src[c * 256:(c + 1) * 256, :].rearrange("(p r) d -> p (r d)", p=128)
        dv = du_s[c * 256:(c + 1) * 256, c0:c0 + D].rearrange("(p r) d -> p r d", p=128)
        dma("pool", TF[:, s_, :], sv, (), [bTF[s_]], "tf%d" % s_)
        cp("pool", TBF[:, s_, :], TF[:, s_, :], [bTF[s_]], [bTBF[s_]])
        dma("pool", dv, TBF[:, s_, :].rearrange("p (r d) -> p r d", r=2), [bTBF[s_]], [bTAB], "tb%d" % s_)

    SR = sb("SR", [128, 2, 4, 128])
    SG = sb("SG", [64, 2, 4, 128])
    bS = [[Buf() for _ in range(8)] for _ in range(2)]
    SI_R = sb("SI_R", [128, 2, 4, 128], F32, scope=p12)
    SI_G = sb("SI_G", [64, 2, 4, 128], F32, scope=p12)
    bSI = [Buf(), Buf()]
    memset("pool", SI_R[:].rearrange("p a b c -> p (a b c)"), 0.0, [bSI[0]])
    memset("pool", SI_G[:].rearrange("p a b c -> p (a b c)"), 0.0, [bSI[1]])
    memset("pool", SR[:].rearrange("p a b c -> p (a b c)"), 0.0, [bS[d_][i] for d_ in range(2) for i in range(4)])
    memset("pool", SG[:].rearrange("p a b c -> p (a b c)"), 0.0, [bS[d_][4 + i] for d_ in range(2) for i in range(4)])
    SBR = sb("SBR", [128, 2, 4, 128], BF16)
    SBG = sb("SBG", [64, 2, 4, 128], BF16)
    bSBT = [Buf(), Buf()]
    bSB = [Buf() for _ in range(OT)]
    SEL = sb("SEL", [128, 4])
    bSEL = Buf()
    dma("sp", SEL[:], segsel, (), [bSEL], "c7")

    XT = sb("XT", [128, 2, D])
    bXT = [Buf(), Buf()]
    ROPE = sb("ROPE", [128, 2, 128])
    bRP = [Buf(), Buf()]
    JUNK = sb("JUNK", [128, D])
    bJUNK = Buf()
    ST = sb("ST", [128, 8])
    bST = Buf()
    XH2 = sb("XH", [128, 1, D])
    bXH2 = [Buf(), Buf()]
    bXH2[1] = bXH2[0]
    HT2 = sb("HT", [128, 2, 8, 128], BF16)
    bHT2 = [Buf(), Buf()]
    KTMP = sb("KTMP", [128, 4, 128])
    RTMP = None
    bKTMP = Buf()
    KDB = sb("KDB", [128, 4, 128], BF16)
    bKDB = Buf()
    RVB = sb("RVB", [128, 512], BF16)
    bRVB = Buf()
    GVB = sb("GVB", [128, 512], BF16)
    bGVB = Buf()
    LRT = sb("LRT", [32, 128], BF16)
    bLRT = Buf()
    LL = sb("LL", [128, 512])
    bLL = Buf()
    EG = sb("EG", [128, 256])
    bEG = Buf()
    KDG = sb("KDG", [128, 256], BF16)
    bKDG = Buf()
    DG = sb("DG", [64, 4])
    bDG = Buf()

    vis = [0]

    def norm_and_transpose(src_ap, ab, rope_ap):
        s = vis[0] % 2
        vis[0] += 1
        XH = XH2[:, 0, :]
        bXH = bXH2[s]
        HT = HT2[:, s]
        bHT = bHT2[s]
        dma("sp", XT[:, s, :], src_ap, (), [bXT[s]], "xt%d" % s)
        if rope_ap is not None:
            dma("act", ROPE[:, s, :], rope_ap, (), [bRP[s]], "rp%d" % s)
        act(XH, XT[:, s, :], AF.Square, [bXT[s]], [bXH, bST], accum=ST[:, 0:1])
        ts("dve", ST[:, 1:2], ST[:, 0:1], 1.0 / D, ALU.mult, [bST], [bST], EPS, ALU.add)
        act(ST[:, 2:3], ST[:, 1:2], AF.Sqrt, [bST], [bST])
        P.op("dve", lambda: nc.vector.reciprocal(out=ST[:, 3:4], in_=ST[:, 2:3]), [bST], [bST])
        ts("dve", XH, XT[:, s, :], ST[:, 3:4], ALU.mult, [bXT[s], bST], [bXH])
        for half in range(2):
            pb = half
            for k4 in range(4):
                k = half * 4 + k4
                tr(psf[pb][:, k4 * 128:(k4 + 1) * 128], XH[:, k * 128:(k + 1) * 128], IDENT, [bXH, bC], [bps[pb]])
            for k4 in range(4):
                k = half * 4 + k4
                if k4 % 2 == 0:
                    act(HT[:, k, :], psf[pb][:, k4 * 128:(k4 + 1) * 128], AF.Identity, [bps[pb], bAB], [bHT],
                        bias=AB1[:, ab + 1, k:k + 1], scale=AB1[:, ab, k:k + 1])
                else:
                    ts("dve", HT[:, k, :], psf[pb][:, k4 * 128:(k4 + 1) * 128], AB1[:, ab, k:k + 1], ALU.mult,
                       [bps[pb], bAB], [bHT], AB1[:, ab + 1, k:k + 1], ALU.add)
        return s, HT, bHT

    def rope_apply(dst, src_ps, s, Rsrc, Wdst):
        x1 = src_ps.rearrange("p (h a b) -> p h a b", h=4, a=2)[:, :, 0, :]
        x2 = src_ps.rearrange("p (h a b) -> p h a b", h=4, a=2)[:, :, 1, :]
        RT = JUNK[:].rearrange("p (a h e) -> p a h e", a=4, h=4)
        cos = ROPE[:, s, 0:64].unsqueeze(1).to_broadcast([128, 4, 64])
        sin = ROPE[:, s, 64:128].unsqueeze(1).to_broadcast([128, 4, 64])
        tt("dve", RT[:, 0], x1, cos, ALU.mult, Rsrc + [bRP[s]], [bJUNK])
        tt("dve", RT[:, 1], x2, sin, ALU.mult, Rsrc + [bRP[s]], [bJUNK])
        tt("dve", RT[:, 2], x2, cos, ALU.mult, Rsrc + [bRP[s]], [bJUNK])
        tt("dve", RT[:, 3], x1, sin, ALU.mult, Rsrc + [bRP[s]], [bJUNK])
        tt("dve", dst[:, :, 0:64], RT[:, 0], RT[:, 1], ALU.subtract, [bJUNK], Wdst)
        tt("dve", dst[:, :, 64:128], RT[:, 2], RT[:, 3], ALU.add, [bJUNK], Wdst)

    def gate_L(d_list, gb=5):
        for d_ in d_list:
            cs_ = slice(d_ * 256, (d_ + 1) * 256)
            mm(psf[gb][:, cs_], LRT[:, :], GKU[:, cs_], True, False, [bLRT, bGK], [bps[gb]])
            mm(psf[gb][:, cs_], ONESR[:, :], GKB[:, cs_], False, True, [bONESR, bGK], [bps[gb]])
            act(LL[:, cs_], psf[gb][:, cs_], AF.Exp, [bps[gb]], [bLL], scale=-1.0)
            act(LL[:, cs_], LL[:, cs_], AF.Ln, [bLL], [bLL], bias=1.0)

    def gla_kdecay(d_, gk_ps, Rgk, kb=6):
        cs_ = slice(d_ * 256, (d_ + 1) * 256)
        m = SGT if d_ == 0 else SLT
        mm(psf[kb][:, 0:256], m, LL[:, cs_], True, True, [bC, bLL], [bps[kb]])
        act(EG[:], psf[kb][:, 0:256], AF.Exp, [bps[kb]], [bEG], scale=-1.0 / 16)
        tt("dve", KDG[:], gk_ps, EG[:], ALU.mult, Rgk + [bEG], [bKDG])
        for h in range(4):
            mm(psf[kb][0:64, 256 + h:257 + h], LL[:, d_ * 256 + h * 64:d_ * 256 + (h + 1) * 64], ONESC,
               True, True, [bLL, bC], [bps[kb]])
        act(DG[:], psf[kb][0:64, 256:260], AF.Exp, [bps[kb]], [bDG], scale=-1.0 / 16)

    def state_update(d_, pbA, pbB):
        for h in range(4):
            mm(psf[pbA][:, h * 128:(h + 1) * 128], KDB[:, h, :], RVB[:, h * 128:(h + 1) * 128], True, True,
               [bKDB, bRVB], [bps[pbA]])
        for h in range(4):
            mm(psf[pbB][0:64, h * 128:(h + 1) * 128], KDG[:, h * 64:(h + 1) * 64], GVB[:, h * 128:(h + 1) * 128],
               True, True, [bKDG, bGVB], [bps[pbB]])
        for h in range(4):
            stt("dve", SR[:, d_, h, :], SR[:, d_, h, :], CD[:, d_ * 4 + h:d_ * 4 + h + 1],
                psf[pbA][:, h * 128:(h + 1) * 128], ALU.mult, ALU.add, [bps[pbA], bDC], [bS[d_][h]])
        for h in range(4):
            stt("dve", SG[:, d_, h, :], SG[:, d_, h, :], DG[:, h:h + 1],
                psf[pbB][0:64, h * 128:(h + 1) * 128], ALU.mult, ALU.add, [bps[pbB], bDG], [bS[d_][4 + h]])

    def state_visit(src_ap, d_, is_ctx, rope_ap):
        P.next_chain()
        table_chunk()
        s, HT, bHT = norm_and_transpose(src_ap, 2 if is_ctx else 0, rope_ap)
        for k in range(8):
            mm(psf[2][:, :], HT[:, k, :], WKV[:, k, 0:512], k == 0, k == 7, [bHT, bWKV], [bps[2]])
        for k in range(8):
            mm(psf[3][:, :], HT[:, k, :], WKV[:, k, 512:1024], k == 0, k == 7, [bHT, bWKV], [bps[3]])
        for k in range(8):
            mm(psf[4][:, 0:256], HT[:, k, :], WKV[:, k, 1024:1280], k == 0, k == 7, [bHT, bWKV], [bps[4]])
        for k in range(8):
            mm(psf[4][0:32, 256:384], WKV[:, k, 1792:1824], HT[:, k, :], k == 0, k == 7, [bHT, bWKV], [bps[4]])
        cp("act", LRT[:], psf[4][0:32, 256:384], [bps[4]], [bLRT])
        kdv = KD[:, d_ * 4:(d_ + 1) * 4].unsqueeze(2).to_broadcast([128, 4, 128])
        if is_ctx:
            tt("dve", KDB[:], psf[2][:, :].rearrange("p (h e) -> p h e", h=4), kdv, ALU.mult,
               [bps[2], bDC], [bKDB])
        else:
            rope_apply(KTMP, psf[2][:, :], s, [bps[2]], [bKTMP])
            tt("dve", KDB[:], KTMP[:], kdv, ALU.mult, [bKTMP, bDC], [bKDB])
        cp("act", RVB[:], psf[3][:, :], [bps[3]], [bRVB])
        for k in range(8):
            mm(psf[3][:, :], HT[:, k, :], WKV[:, k, 1280:1792], k == 0, k == 7, [bHT, bWKV], [bps[3]])
        cp("act", GVB[:], psf[3][:, :], [bps[3]], [bGVB])
        gate_L([d_])
        gla_kdecay(d_, psf[4][:, 0:256], [bps[4]])
        state_update(d_, 5, 6)

    def snapshot(d_, sidx):
        for h in range(4):
            stt("dve", SI_R[:, d_, h, :], SR[:, d_, h, :], SEL[:, sidx:sidx + 1], SI_R[:, d_, h, :],
                ALU.mult, ALU.add, [bS[d_][h], bSEL], [bSI[0]])
            stt("dve", SI_G[:, d_, h, :], SG[:, d_, h, :], SEL[0:64, sidx:sidx + 1], SI_G[:, d_, h, :],
                ALU.mult, ALU.add, [bS[d_][4 + h], bSEL], [bSI[1]])

    def xb_tile(i):
        return xb[i * 128:(i + 1) * 128, :]

    P.begin_chains()
    if n_state:
        state_visit(ctx[0:128, :], 0, True, None)
        state_visit(ctx[128:256, :], 0, True, None)
        snapshot(0, 0)
        for i in range(48):
            state_visit(xb_tile(i), 0, False, ropeb[i * 128:(i + 1) * 128, :])
            if i % 16 == 15:
                snapshot(0, i // 16 + 1)
        state_visit(ctx[128:256, :], 1, True, None)
        state_visit(ctx[0:128, :], 1, True, None)
        snapshot(1, 3)
        for i in range(63, 15, -1):
            state_visit(xb_tile(i), 1, False, ropeb[i * 128:(i + 1) * 128, :])
            if i % 16 == 0:
                snapshot(1, i // 16 - 1)
    for h in range(4):
        cp("dve", SR[:, 1, h, :], SI_R[:, 1, h, :], [bSI[0]], [bS[1][h]])
        cp("dve", SG[:, 1, h, :], SI_G[:, 1, h, :], [bSI[1]], [bS[1][4 + h]])
    for c in range(n_own - 1, -1, -1):
        for h in range(4):
            cp("pool", SBR[:, c % 2, h, :], SR[:, 1, h, :], [bS[1][h]], [bSBT[c % 2]])
            cp("pool", SBG[:, c % 2, h, :], SG[:, 1, h, :], [bS[1][4 + h]], [bSBT[c % 2]])
        dma("sp", sbr_s[c], SBR[:, c % 2].rearrange("p a b -> p (a b)"), [bSBT[c % 2]], [bSB[c]], "sbw%d" % (c % 2))
        dma("sp", sbg_s[c], SBG[:, c % 2].rearrange("p a b -> p (a b)"), [bSBT[c % 2]], [bSB[c]], "sbw%d" % (c % 2))
        if c > 0:
            state_visit(xo[c * 128:(c + 1) * 128, :], 1, False, ropeo[c * 128:(c + 1) * 128, :])
    for h in range(4):
        cp("dve", SR[:, 0, h, :], SI_R[:, 0, h, :], [bSI[0]], [bS[0][h]])
        cp("dve", SG[:, 0, h, :], SI_G[:, 0, h, :], [bSI[1]], [bS[0][4 + h]])

    while tconv[0] < 128:
        table_chunk()
    P.end_chains(6)
    P.barrier()
    print("sbuf after pass2", nc.sbuf_bytes_remaining)
    p12.close()
    p3 = ExitStack()
    NG = 12
    UV = sb("UV", [128, NG, 2 * D], BF16, p3)
    bUV = [Buf() for _ in range(NG)]
    DGS = sb("DGS", [128, 4, 128], BF16, p3)
    bDGS = [Buf() for _ in range(4)]
    JB = sb("JB", [128, D], BF16, p3)
    bJB = Buf()
    ACTV = sb("ACTV", [128, 128], F32, p3)
    bA = [Buf() for _ in range(128)]
    bCf = [Buf() for _ in range(64)]
    COEF = sb("COEF", [128, 128], F32, p3)
    ACC = sb("ACC", [128, D], F32, p3)
    bACC = Buf()
    ST2 = sb("ST2", [128, 8], F32, p3)
    bST2 = Buf()
    X1D = sb("X1D", [128, 2, D], F32, p3)
    bX1 = [Buf(), Buf()]
    H2BD = sb("H2BD", [128, 2, D], BF16, p3)
    bH2B = [Buf(), Buf()]
    IDXD = sb("IDXD", [128, 2, 128], I32, p3)
    bIDX = [Buf(), Buf()]
    GWD = sb("GWD", [128, 2, 128], F32, p3)
    bGW = [Buf(), Buf()]
    finals = []
    wcnt = [0]

    def wload(src, n):
        s = wcnt[0] % 3
        wcnt[0] += 1
        dma("sp", WB[:, s, :, 0:n], src.rearrange("(k p) n -> p k n", p=128), bWSall, [bWB[s]], "wb%d" % s)
        return s

    def mix(c):
        pc = c % 2
        mx = ExitStack()
        bl = []

        def nb():
            b_ = P.newbuf()
            bl.append(b_)
            return b_
        QR = sb("QR", [128, 4, 128], BF16, mx)
        KR = sb("KR", [128, 4, 128], BF16, mx)
        bQR = nb()
        bKR = nb()
        GQK = sb("GQK", [128, 512], BF16, mx)
        bGQK = nb()
        SGATE = sb("SGATE", [128, D], F32, mx)
        bSGATE = nb()
        QKT = sb("QKT", [128, 8, 128], BF16, mx)
        bQKT = nb()
        GT = sb("GT", [64, 8, 128], BF16, mx)
        bGT = nb()
        EQK = sb("EQK", [64, 2, 4, 128], F32, mx)
        bEQK = nb()
        QKTT = sb("QKTT", [64, 2, 2, 4, 128], BF16, mx)
        bQKTT = [nb(), nb()]
        SM = sb("SM", [128, 4, 128], BF16, mx)
        bSM = [nb() for _ in range(4)]
        QD = sb("QD", [128, 4, 128], BF16, mx)
        bQD = [nb() for _ in range(4)]
        SBF = sb("SBF", [128, 8, 128], BF16, mx)
        bSBF = nb()
        YB = sb("YB", [128, D], BF16, mx)
        bYB = nb()
        YT = sb("YT", [128, 8, 128], BF16, mx)
        bYT = nb()
        SBRc = sb("SBRc", [128, 4, 128], BF16, mx)
        SBGc = sb("SBGc", [64, 4, 128], BF16, mx)
        bSBc = nb()
        dma("sp", SBRc[:].rearrange("p a b -> p (a b)"), sbr_s[c], [bSB[c]], [bSBc], "sbl")
        dma("sp", SBGc[:].rearrange("p a b -> p (a b)"), sbg_s[c], [bSB[c]], [bSBc], "sbl")
        s, HT, bHT = norm_and_transpose(xo[c * 128:(c + 1) * 128, :], 0, ropeo[c * 128:(c + 1) * 128, :])
        for h in range(4):
            cp("act", SBF[:, h, :], SR[:, 0, h, :], [bS[0][h]], [bSBF])
            cp("act", SBF[0:64, 4 + h, :], SG[:, 0, h, :], [bS[0][4 + h]], [bSBF])
        for g in range(8):
            n = 512 if g < 7 else 32
            ws = wload(w_in_s[:, g * 512:g * 512 + n], n)
            pb = 2 + g % 2
            if g < 7:
                for k in range(8):
                    mm(psf[pb][:, :], HT[:, k, :], WB[:, ws, k, :], k == 0, k == 7, [bHT, bWB[ws]], [bps[pb]])
            else:
                for k in range(8):
                    mm(psf[pb][0:32, 0:128], WB[:, ws, k, 0:32], HT[:, k, :], k == 0, k == 7, [bHT, bWB[ws]], [bps[pb]])
            pv = psf[pb][:, :]
            if g == 0:
                rope_apply(KTMP, pv, s, [bps[pb]], [bKTMP])
                cp("act", QR[:], KTMP[:], [bKTMP], [bQR])
            elif g == 1:
                rope_apply(KTMP, pv, s, [bps[pb]], [bKTMP])
                cp("act", KR[:], KTMP[:], [bKTMP], [bKR])
                tt("dve", KDB[:], KTMP[:], KD[:, 0:4].unsqueeze(2).to_broadcast([128, 4, 128]), ALU.mult,
                   [bKTMP, bDC], [bKDB])
            elif g == 2:
                cp("act", RVB[:], pv, [bps[pb]], [bRVB])
            elif g == 3:
                act(SGATE[:, 0:512], pv, AF.Silu, [bps[pb]], [bSGATE])
            elif g == 4:
                cp("act", GQK[:], pv, [bps[pb]], [bGQK])
            elif g == 5:
                cp("act", GVB[:], pv, [bps[pb]], [bGVB])
            elif g == 6:
                act(SGATE[:, 512:1024], pv, AF.Silu, [bps[pb]], [bSGATE])
            else:
                cp("act", LRT[:], psf[pb][0:32, 0:128], [bps[pb]], [bLRT])
        for h in range(4):
            tr(psb[:, h * 128:(h + 1) * 128], QR[:, h, :], IDB[:], [bQR, bIDB], [bpsb])
            tr(psb[:, (4 + h) * 128:(5 + h) * 128], KR[:, h, :], IDB[:], [bKR, bIDB], [bpsb])
        cp("act", QKT[:].rearrange("p a b -> p (a b)"), psb[:, :], [bpsb], [bQKT])
        for h in range(8):
            tr(psb[0:64, h * 128:(h + 1) * 128], GQK[:, h * 64:(h + 1) * 64], IDB[:], [bGQK, bIDB], [bpsb])
        cp("act", GT[:].rearrange("p a b -> p (a b)"), psb[0:64, :], [bpsb], [bGT])
        gate_L([0, 1], 4)
        for d_ in range(2):
            m = SLE if d_ == 0 else SGE
            pb = d_
            for h in range(4):
                mm(psf[pb][0:64, h * 128:(h + 1) * 128], LL[:, d_ * 256 + h * 64:d_ * 256 + (h + 1) * 64], m,
                   True, True, [bLL, bC], [bps[pb]])
            act(EQK[:, 0].rearrange("p a b -> p (a b)"), psf[pb][0:64, :], AF.Exp, [bps[pb]], [bEQK],
                scale=-1.0 / 16)
            act(EQK[:, 1].rearrange("p a b -> p (a b)"), psf[pb][0:64, :], AF.Exp, [bps[pb]], [bEQK],
                scale=1.0 / 16)
            stt("dve", QKTT[:, d_, 0].rearrange("p a b -> p (a b)"), GT[:, 0:4, :].rearrange("p a b -> p (a b)"),
                0.125, EQK[:, 0].rearrange("p a b -> p (a b)"), ALU.mult, ALU.mult, [bGT, bEQK], [bQKTT[d_]])
            tt("dve", QKTT[:, d_, 1].rearrange("p a b -> p (a b)"), GT[:, 4:8, :].rearrange("p a b -> p (a b)"),
               EQK[:, 1].rearrange("p a b -> p (a b)"), ALU.mult, [bGT, bEQK], [bQKTT[d_]])
        gla_kdecay(0, GQK[:, 256:512], [bGQK], 4)
        rot = 0
        for h in range(4):
            oreg = psf[2][:, h * 128:(h + 1) * 128]
            for d_ in range(2):
                c_ = d_ * 4 + h
                r = rot % 4
                rot += 1
                scp = psf[4][:, r * 128:(r + 1) * 128]
                mm(scp, QKT[:, 4 + h, :], QKT[:, h, :], True, True, [bQKT], [bps[4]])
                tt("dve", SM[:, r, :], scp, MASK[:, c_, :], ALU.mult, [bps[4], bDC], [bSM[r]])
                mm(oreg, SM[:, r, :], RVB[:, h * 128:(h + 1) * 128], d_ == 0, False, [bSM[r], bRVB], [bps[2]])
                tt("dve", QD[:, r, :], QKT[:, h, :], QDC[:, c_, :], ALU.mult, [bQKT, bDC], [bQD[r]])
                if d_ == 0:
                    mm(oreg, QD[:, r, :], SBF[:, h, :], False, False, [bQD[r], bSBF], [bps[2]])
                else:
                    mm(oreg, QD[:, r, :], SBRc[:, h, :], False, True, [bQD[r], bSBc], [bps[2]])
        for h in range(4):
            oreg = psf[3][:, h * 128:(h + 1) * 128]
            for d_ in range(2):
                r = rot % 4
                rot += 1
                scp = psf[4][:, r * 128:(r + 1) * 128]
                mm(scp, QKTT[:, d_, 1, h, :], QKTT[:, d_, 0, h, :], True, True, [bQKTT[d_]], [bps[4]])
                tt("dve", SM[:, r, :], scp, SLE if d_ == 0 else SGE, ALU.mult, [bps[4], bC], [bSM[r]])
                mm(oreg, SM[:, r, :], GVB[:, h * 128:(h + 1) * 128], d_ == 0, False, [bSM[r], bGVB], [bps[3]])
                if d_ == 0:
                    mm(oreg, QKTT[:, d_, 0, h, :], SBF[0:64, 4 + h, :], False, False, [bQKTT[d_], bSBF], [bps[3]])
                else:
                    mm(oreg, QKTT[:, d_, 0, h, :], SBGc[:, h, :], False, True, [bQKTT[d_], bSBc], [bps[3]])
        state_update(0, 0, 1)
        for half in range(2):
            pv = psf[2 + half][:, :]
            act(JUNK[:, half * 512:(half + 1) * 512], pv, AF.Square, [bps[2 + half]], [bJUNK])
        red("dve", ST[:, 0:8], JUNK[:].rearrange("p (h e) -> p h e", h=8), ALU.add, [bJUNK], [bST])
        ts("dve", ST[:, 0:8], ST[:, 0:8], 1.0 / 128, ALU.mult, [bST], [bST], EPS, ALU.add)
        act(ST[:, 0:8], ST[:, 0:8], AF.Sqrt, [bST], [bST])
        P.op("dve", lambda: nc.vector.reciprocal(out=ST[:, 0:8], in_=ST[:, 0:8]), [bST], [bST])
        for half in range(2):
            tt("dve", JUNK[:, half * 512:(half + 1) * 512].rearrange("p (h e) -> p h e", h=4),
               psf[2 + half][:, :].rearrange("p (h e) -> p h e", h=4),
               ST[:, half * 4:(half + 1) * 4].unsqueeze(2).to_broadcast([128, 4, 128]), ALU.mult,
               [bps[2 + half], bST], [bJUNK])
        tt("dve", YB[:], JUNK[:], SGATE[:], ALU.mult, [bJUNK, bSGATE], [bYB])
        for k in range(8):
            tr(psb[:, k * 128:(k + 1) * 128], YB[:, k * 128:(k + 1) * 128], IDB[:], [bYB, bIDB], [bpsb])
        cp("act", YT[:].rearrange("p a b -> p (a b)"), psb[:, :], [bpsb], [bYT])
        for g in range(2):
            ws = wload(w_out_s[:, g * 512:(g + 1) * 512], 512)
            for k in range(8):
                mm(psf[g][:, :], YT[:, k, :], WB[:, ws, k, :], k == 0, k == 7, [bYT, bWB[ws]], [bps[g]])
            tt("dve", X1D[:, pc, g * 512:(g + 1) * 512], psf[g][:, :], XT[:, s, g * 512:(g + 1) * 512], ALU.add,
               [bps[g], bXT[s]], [bX1[pc]])
        if stage == "x1":
            finals.append(dma("sp", out_d[c * 128:(c + 1) * 128, :], X1D[:, pc, :], [bX1[pc]], (), "out"))
        P.scope_fence(bl)
        mx.close()
        if stage == "x1":
            return
        pr = ExitStack()
        bl = []
        H2 = sb("H2", [128, D], F32, pr)
        bH2 = nb()
        IDXF_ = None
        bIDXF = nb()
        GWV = GWD[:, pc, :].rearrange("p (a b) -> p a b", a=8)
        YT = sb("H2T", [128, 8, 128], BF16, pr)
        bYT = nb()
        QT = sb("QT", [128, 16, 128], BF16, pr)
        bQT = nb()
        S12U = sb("S12U", [128, 2048], F32, pr)
        bS12 = nb()
        bSg = [nb() for _ in range(16)]
        bVg = [nb() for _ in range(16)]
        bIg = [nb() for _ in range(16)]
        bTh = [nb() for _ in range(8)]
        bPh = [nb() for _ in range(8)]
        bCAND = bS12
        bOH = bS12
        S12 = S12U[:].rearrange("p (a b) -> p a b", a=16)
        CAND = S12U[:].rearrange("p (a b) -> p a b", a=8)
        OH = S12U[:].rearrange("p (h a b) -> p h a b", h=8, a=16)
        SCR = sb("SCR", [128, 2, 256], F32, pr)
        bSCR = [nb(), nb()]
        V12 = sb("V12", [128, 16, 16], F32, pr)
        I12 = sb("I12", [128, 16, 16], U32, pr)
        I12F = sb("I12F", [128, 16, 16], F32, pr)
        bV12 = nb()
        TSV = sb("TSV", [128, 8, 16], F32, pr)
        POS = sb("POS", [128, 8, 16], U32, pr)
        PAF = sb("PAF", [128, 3, 8, 16], F32, pr)
        bTS = nb()
        ISEL = sb("ISEL", [128, 2, 8, 16], F32, pr)
        IDXF = sb("IDXF", [128, 128], F32, pr)
        act(JUNK[:], X1D[:, pc, :], AF.Square, [bX1[pc]], [bJUNK, bST], accum=ST[:, 0:1])
        ts("dve", ST[:, 1:2], ST[:, 0:1], 1.0 / D, ALU.mult, [bST], [bST], EPS, ALU.add)
        act(ST[:, 2:3], ST[:, 1:2], AF.Sqrt, [bST], [bST])
        P.op("dve", lambda: nc.vector.reciprocal(out=ST[:, 3:4], in_=ST[:, 2:3]), [bST], [bST])
        stt("dve", H2[:], X1D[:, pc, :], ST[:, 3:4], A2, ALU.mult, ALU.mult, [bX1[pc], bST, bROWS], [bH2])
        tt("dve", H2[:], H2[:], B2, ALU.add, [bH2, bROWS], [bH2])
        cp("act", H2BD[:, pc, :], H2[:], [bH2], [bH2B[pc]])
        for k in range(8):
            tr(psb[:, k * 128:(k + 1) * 128], H2BD[:, pc, k * 128:(k + 1) * 128], IDB[:], [bH2B[pc], bIDB], [bpsb])
        cp("act", YT[:].rearrange("p a b -> p (a b)"), psb[:, :], [bpsb], [bYT])
        for g4 in range(4):
            ws = wload(w_q_s[:, g4 * 512:(g4 + 1) * 512], 512)
            pb = 2 + g4 % 2
            for gg in range(4):
                for k in range(8):
                    mm(psf[pb][:, gg * 128:(gg + 1) * 128], WB[:, ws, k, gg * 128:(gg + 1) * 128], YT[:, k, :],
                       k == 0, k == 7, [bYT, bWB[ws]], [bps[pb]])
            cp("act", QT[:, g4 * 4:(g4 + 1) * 4, :].rearrange("p a b -> p (a b)"),
               psf[pb][:, :], [bps[pb]], [bQT])
        for g4 in range(4):
            pb = g4 % 2
            for gg in range(4):
                g = g4 * 4 + gg
                mm(psf[pb][:, gg * 128:(gg + 1) * 128], QT[:, g, :], KT[:, g, :], True, True, [bQT, bKT], [bps[pb]])
            cp("act", S12U[:, g4 * 512:(g4 + 1) * 512],
               psf[pb][:, :], [bps[pb]], [bS12] + bSg[g4 * 4:(g4 + 1) * 4])
        for g in range(16):
            P.op("dve", (lambda g=g: nc.vector.max(out=V12[:, g, 0:8], in_=S12[:, g, :])), [bSg[g]], [bVg[g]])
        for g in range(16):
            P.op("dve", (lambda g=g: nc.vector.max_index(out=I12[:, g, 0:8], in_max=V12[:, g, 0:8], in_values=S12[:, g, :])),
                 [bSg[g], bVg[g]], [bIg[g]])
        for g in range(16):
            P.op("dve", (lambda g=g: nc.vector.match_replace(out=S12[:, g, :], in_to_replace=V12[:, g, 0:8],
                                                             in_values=S12[:, g, :], imm_value=NEG)),
                 [bVg[g]], [bSg[g]])
        for g in range(16):
            P.op("dve", (lambda g=g: nc.vector.max(out=V12[:, g, 8:16], in_=S12[:, g, :])), [bSg[g]], [bVg[g]])
        for g in range(16):
            P.op("dve", (lambda g=g: nc.vector.max_index(out=I12[:, g, 8:16], in_max=V12[:, g, 8:16],
                                                         in_values=S12[:, g, :])), [bSg[g], bVg[g]], [bIg[g]])
        cp("dve", I12F[:].rearrange("p a b -> p (a b)"), I12[:].rearrange("p a b -> p (a b)"), bIg, [bV12])
        V3 = V12[:].rearrange("p (h two) k -> p h two k", two=2)
        tt("dve", OH,
           V3[:, :, 0, :].unsqueeze(3).to_broadcast([128, 8, 16, 16]),
           V3[:, :, 1, :].unsqueeze(2).to_broadcast([128, 8, 16, 16]), ALU.add, bVg, bSg)
        bCh = [[bSg[2 * h], bSg[2 * h + 1]] for h in range(8)]
        for h in range(8):
            P.op("dve", (lambda h=h: nc.vector.max(out=TSV[:, h, 0:8], in_=CAND[:, h, :])), bCh[h], [bTh[h]])
        for h in range(8):
            P.op("dve", (lambda h=h: nc.vector.max_index(out=POS[:, h, 0:8], in_max=TSV[:, h, 0:8], in_values=CAND[:, h, :])),
                 bCh[h] + [bTh[h]], [bPh[h]])
        for h in range(8):
            P.op("dve", (lambda h=h: nc.vector.match_replace(out=CAND[:, h, :], in_to_replace=TSV[:, h, 0:8],
                                                             in_values=CAND[:, h, :], imm_value=NEG)),
                 [bTh[h]], bCh[h])
        for h in range(8):
            P.op("dve", (lambda h=h: nc.vector.max(out=TSV[:, h, 8:16], in_=CAND[:, h, :])), bCh[h], [bTh[h]])
        for h in range(8):
            P.op("dve", (lambda h=h: nc.vector.max_index(out=POS[:, h, 8:16], in_max=TSV[:, h, 8:16],
                                                         in_values=CAND[:, h, :])), bCh[h] + [bTh[h]], [bPh[h]])
        P.op("dve", (lambda: nc.vector.tensor_copy(out=PAF[:, 2].rearrange("p a b -> p (a b)"),
                                                   in_=POS[:].rearrange("p a b -> p (a b)"))),
             bPh + bTh + bSg, [bTS, bS12], cost=0.3)
        tt("dve", OH, PAF[:, 2].unsqueeze(3).to_broadcast([128, 8, 16, 16]),
           THR16.unsqueeze(1).unsqueeze(1).to_broadcast([128, 8, 16, 16]), ALU.is_ge, [bTS, bC], [bOH])
        red("dve", PAF[:, 0], OH, ALU.add, [bOH], [bTS])
        stt("dve", PAF[:, 1].rearrange("p a b -> p (a b)"), PAF[:, 0].rearrange("p a b -> p (a b)"), -16.0,
            PAF[:, 2].rearrange("p a b -> p (a b)"), ALU.mult, ALU.add, [bTS], [bTS])
        I3 = I12F[:].rearrange("p (h two) k -> p h two k", two=2)
        for w in range(2):
            tt("dve", OH, PAF[:, w].unsqueeze(3).to_broadcast([128, 8, 16, 16]),
               IOTA16.unsqueeze(1).unsqueeze(1).to_broadcast([128, 8, 16, 16]), ALU.is_equal, [bTS, bC], [bOH])
            tt("dve", OH, OH, I3[:, :, w, :].unsqueeze(2).to_broadcast([128, 8, 16, 16]), ALU.mult,
               [bOH, bV12], [bOH])
            red("dve", ISEL[:, w], OH, ALU.add, [bOH], [bTS])
        stt("dve", IDXF[:], ISEL[:, 0].rearrange("p a b -> p (a b)"), 128.0, ISEL[:, 1].rearrange("p a b -> p (a b)"),
            ALU.mult, ALU.add, [bTS], [bIDXF])
        cp("dve", IDXD[:, pc, :], IDXF[:], [bIDXF], [bIDX[pc]])
        tt("dve", GWV, TSV[:], TSV[:, :, 0:1].to_broadcast([128, 8, 16]), ALU.subtract, [bTS], [bGW[pc]])
        act(GWD[:, pc, :], GWD[:, pc, :], AF.Exp, [bGW[pc]], [bGW[pc]])
        red("dve", ST[:, 0:8], GWV, ALU.add, [bGW[pc]], [bST])
        P.op("dve", lambda: nc.vector.reciprocal(out=ST[:, 0:8], in_=ST[:, 0:8]), [bST], [bST])
        tt("dve", GWV, GWV, ST[:, 0:8].unsqueeze(2).to_broadcast([128, 8, 16]), ALU.mult, [bGW[pc], bST], [bGW[pc]])
        P.scope_fence(bl)
        pr.close()

    def gather(c):
        pc = c % 2
        LA = []
        LB = []
        BLK = 4
        nblk = 128 // BLK
        assert NG >= 2 * BLK + 1
        P.insts = LA
        memset("pool", ACTV[:], 0.0, bA)
        GW2 = GWD[:, pc, :]
        for b_ in range(nblk):
            P.insts = LA
            for k in range(BLK):
                j = b_ * BLK + k
                r = j % NG
                P.op("pool", (lambda j=j, r=r: nc.gpsimd.indirect_dma_start(
                    out=UV[:, r, :], out_offset=None, in_=du_s,
                    in_offset=bass.IndirectOffsetOnAxis(ap=IDXD[:, pc, j:j + 1], axis=0))),
                    [bIDX[pc], bTAB], [bUV[r]], dma=True, slot="uv%d" % r, cost=1.25, lat=5.0)
                stt("dve", JB[:], UV[:, r, 0:D], 1.0, H2BD[:, pc, :], ALU.mult, ALU.mult,
                    [bUV[r], bH2B[pc]], [bJB, bA[j]], accum=ACTV[:, j:j + 1])
            P.insts = LB
            js = slice(b_ * BLK, (b_ + 1) * BLK)
            act(COEF[:, js], ACTV[:, js], AF.Gelu, [bA[b_ * BLK + k] for k in range(BLK)], [bCf[b_]])
            tt("dve", COEF[:, js], COEF[:, js], GW2[:, js], ALU.mult, [bCf[b_], bGW[pc]], [bCf[b_]])
            for k in range(BLK):
                j = b_ * BLK + k
                r = j % NG
                dsl = j % 4
                act(DGS[:, dsl, :], IDB[:], AF.Copy, [bIDB, bCf[b_]], [bDGS[dsl]], scale=COEF[:, j:j + 1])
                for g in range(2):
                    mm(psf[5 + g][:, :], DGS[:, dsl, :], UV[:, r, D + g * 512:D + (g + 1) * 512],
                       j == 0, j == 127, [bDGS[dsl], bUV[r]], [bps[5 + g]])
        P.insts = LB
        for g in range(2):
            tt("dve", ACC[:, g * 512:(g + 1) * 512], psf[5 + g][:, :], G2[:, g * 512:(g + 1) * 512], ALU.mult,
               [bps[5 + g], bROWS], [bACC])
        tt("dve", ACC[:], ACC[:], X1D[:, pc, :], ALU.add, [bACC, bX1[pc]], [bACC])
        act(JB[:], ACC[:], AF.Square, [bACC], [bJB, bST2], accum=ST2[:, 0:1])
        ts("dve", ST2[:, 1:2], ST2[:, 0:1], 1.0 / D, ALU.mult, [bST2], [bST2], EPS, ALU.add)
        act(ST2[:, 2:3], ST2[:, 1:2], AF.Sqrt, [bST2], [bST2])
        P.op("dve", lambda: nc.vector.reciprocal(out=ST2[:, 3:4], in_=ST2[:, 2:3]), [bST2], [bST2])
        stt("dve", ACC[:], ACC[:], ST2[:, 3:4], NF, ALU.mult, ALU.mult, [bACC, bST2, bROWS], [bACC])
        finals.append(dma("sp", out_d[c * 128:(c + 1) * 128, :], ACC[:], [bACC], (), "out"))
        for L_ in (LA, LB):
            P._chains.append(L_)
            P._bias.append(0.0)
        P.insts = []

    P.begin_chains()
    P.next_chain(MIXBIAS)
    mix(0)
    print("sbuf in pass3", nc.sbuf_bytes_remaining)
    if stage != "x1":
        for c in range(n_own):
            P.next_chain(0.0)
            gather(c)
            P.next_chain(MIXBIAS)
            if c + 1 < n_own:
                mix(c + 1)
    else:
        for c in range(1, n_own):
            P.next_chain()
            mix(c)
    P.end_chains(9)

    P.barrier()
    p3.close()
    es.close()
    return nc, P.stats


def _consts():
    p = np.arange(128, dtype=np.float32)
    J = p[:, None]
    I = p[None, :]
    cmats = np.stack([
        np.eye(128, dtype=np.float32),
        (J <= I).astype(np.float32),
        (J >= I).astype(np.float32),
        np.maximum(I - J, 0.0),
        np.maximum(J - I, 0.0),
        (J > I).astype(np.float32),
        (J < I).astype(np.float32),
        np.broadcast_to(I + 1.0, (128, 128)),
    ], axis=1).astype(np.float32)
    csm = np.zeros((128, 48), np.float32)
    csm[:, 0:4] = (127.0 - p)[:, None]
    csm[:, 4:8] = p[:, None]
    csm[:, 8:24] = np.arange(16, dtype=np.float32)[None, :]
    csm[:, 24] = 1.0
    csm[:, 32:48] = 16.0 * (np.arange(16, dtype=np.float32) + 1.0)[None, :]
    r, c = np.meshgrid(np.arange(128, dtype=np.float32), np.arange(64, dtype=np.float32), indexing="ij")
    inv = (10000.0 ** (-np.arange(32, dtype=np.float32) / 32)).astype(np.float32)
    ang = np.concatenate([r.reshape(-1, 1) * inv, c.reshape(-1, 1) * inv], axis=-1).astype(np.float32)
    rope = np.concatenate([np.cos(ang), np.sin(ang)], axis=-1).astype(np.float32)
    return cmats.reshape(128, 1024), csm, rope


def make_in_maps(inputs):
    f = lambda a: np.ascontiguousarray(np.asarray(a, dtype=np.float32))
    x = f(inputs["x"]); c = f(inputs["c"]); ctx = f(inputs["ctx"]); c_ctx = f(inputs["c_ctx"])
    cm, csm, rope = _consts()
    w_mod = f(inputs["w_mod"][0]); b_mod = f(inputs["b_mod"][0])
    bmodT = f(b_mod.reshape(48, 128).T)
    n1T = f(np.asarray(inputs["norm1_w"][0]).reshape(8, 128).T)
    n2row = f(np.asarray(inputs["norm2_w"][0]).reshape(1, D))
    nfrow = f(np.asarray(inputs["norm_f_w"]).reshape(1, D))
    w_in = f(inputs["w_in"][0])
    retd = f(np.concatenate([np.asarray(inputs["ret_decay_f"][0]), np.asarray(inputs["ret_decay_b"][0])]).reshape(1, 8))
    gkup = np.zeros((32, 512), np.float32)
    gkup[0:16, 0:256] = np.asarray(inputs["gla_gk_up_f"][0])
    gkup[16:32, 256:512] = np.asarray(inputs["gla_gk_up_b"][0])
    gkbias = f(np.concatenate([np.asarray(inputs["gla_gk_bias_f"][0]), np.asarray(inputs["gla_gk_bias_b"][0])]).reshape(1, 512))
    w_out = f(inputs["w_out"][0]); w_q = f(inputs["peer_w_q"][0])
    k1 = np.asarray(inputs["peer_k1"][0]); k2 = np.asarray(inputs["peer_k2"][0])
    k12 = np.stack([k1, k2], axis=1).reshape(16, 128, 128)
    k12T = f(k12.transpose(2, 0, 1).reshape(128, 2048))
    down = f(inputs["peer_down"][0]); up = f(inputs["peer_up"][0])
    maps = []
    for core in range(8):
        b, seg = core // 4, core % 4
        cT = f(np.concatenate([c[b].reshape(8, 128).T, c_ctx.reshape(8, 128).T], axis=1))
        sel = np.zeros((128, 4), np.float32)
        sel[:, seg] = 1.0
        maps.append(dict(
            xb=x[b], xo=f(x[b, seg * 2048:(seg + 1) * 2048]), ctx=ctx[b], cT=cT, w_mod=w_mod, bmodT=bmodT,
            n1T=n1T, n2row=n2row, nfrow=nfrow, w_in=w_in, retd=retd, gkup=gkup, gkbias=gkbias,
            w_out=w_out, w_q=w_q, k12T=k12T, down=down, up=up, cm=cm, cs=csm,
            ropeb=rope, ropeo=f(rope[seg * 2048:(seg + 1) * 2048]), segsel=sel))
    return maps


def kernel(**inputs):
    nc, _ = build("full")
    maps = make_in_maps(inputs)
    res = run_bass_kernel_spmd(nc, maps, core_ids=list(range(8)))
    out = np.zeros((2, SEQ, D), np.float32)
    for core in range(8):
        b, seg = core // 4, core % 4
        out[b, seg * 2048:(seg + 1) * 2048] = res.results[core]["out"]
    return out
```
